# Optimizing a Trainium2 kernel written in Bass

```python
import jax, jax.numpy as jnp
from jax import lax
import numpy as np

D_MODEL = 1024
BATCH = 4
SEQ = 4096
DEPTH = 2

HEAD_DIM = 64
N_Q_HEADS = 8
N_KV_HEADS = 2
MIX_WIDTH = N_Q_HEADS * HEAD_DIM
KV_WIDTH = N_KV_HEADS * HEAD_DIM
N_MIXERS = 3
D_FF = 4 * D_MODEL
ROPE_THETA = 10000.0
EPS = 1e-6
NEG_INF = -1e30
Q_BLOCK = 128

IDX_HEADS = 4
IDX_DIM = 64
DSA_TOPK_MAX = 256

SWA_WINDOW = 128

CMP_LEN = 32
CMP_STRIDE = 16
CMP_HIDDEN = 256
SLC_LEN = 64
SLC_TOPN = 16
SLC_Q_BLOCK = 64
NSA_WINDOW = 512
FORCE_SCORE = 1e9

IN_SIZES = (
    MIX_WIDTH, KV_WIDTH, KV_WIDTH, IDX_HEADS * IDX_DIM, IDX_DIM, IDX_HEADS,
    MIX_WIDTH, KV_WIDTH, KV_WIDTH,
    MIX_WIDTH, KV_WIDTH, KV_WIDTH, KV_WIDTH, KV_WIDTH, KV_WIDTH, KV_WIDTH, 3 * N_Q_HEADS,
    N_MIXERS * D_MODEL,
)
D_IN = sum(IN_SIZES)
IN_OFFSETS = tuple(int(o) for o in np.cumsum(IN_SIZES)[:-1])

kernel_name = "hybrid_dsa_swa_nsa_block"


def rms_norm(x, g):
    xf = x.astype(jnp.float32)
    y = xf * lax.rsqrt(jnp.mean(xf * xf, axis=-1, keepdims=True) + EPS)
    return (y * g.astype(jnp.float32)).astype(x.dtype)


def rope_tables(seq_len, dim):
    inv_freq = ROPE_THETA ** (-jnp.arange(0, dim, 2, dtype=jnp.float32) / dim)
    ang = jnp.arange(seq_len, dtype=jnp.float32)[:, None] * inv_freq[None, :]
    return jnp.cos(ang), jnp.sin(ang)


def apply_rope(x, cos, sin):
    c = cos[None, :, None, :].astype(x.dtype)
    s = sin[None, :, None, :].astype(x.dtype)
    x1, x2 = jnp.split(x, 2, axis=-1)
    return jnp.concatenate([x1 * c - x2 * s, x2 * c + x1 * s], axis=-1)


def masked_softmax(s, mask):
    p = jax.nn.softmax(jnp.where(mask, s.astype(jnp.float32), NEG_INF), axis=-1)
    return p * mask.astype(jnp.float32)


def to_blocks(t, n_blocks):
    return jnp.swapaxes(t.reshape(t.shape[0], n_blocks, -1, *t.shape[2:]), 0, 1)


def from_blocks(t):
    t = jnp.swapaxes(t, 0, 1)
    return t.reshape(t.shape[0], -1, *t.shape[3:])


def banded_attention(q, k, v, window, sinks):
    B, T, H, D = q.shape
    Hkv = k.shape[2]
    G = H // Hkv
    nb = T // Q_BLOCK
    n_prev = (window + Q_BLOCK - 2) // Q_BLOCK
    pad = n_prev * Q_BLOCK
    width = pad + Q_BLOCK
    kp = jnp.pad(k, ((0, 0), (pad, 0), (0, 0), (0, 0)))
    vp = jnp.pad(v, ((0, 0), (pad, 0), (0, 0), (0, 0)))

    def band(t):
        return jnp.concatenate(
            [t[:, j * Q_BLOCK: j * Q_BLOCK + T].reshape(B, nb, Q_BLOCK, Hkv, D) for j in range(n_prev + 1)], axis=2)

    kb, vb = band(kp), band(vp)
    qb = q.reshape(B, nb, Q_BLOCK, Hkv, G, D)
    r = jnp.arange(Q_BLOCK)[:, None]
    c = jnp.arange(width)[None, :]
    rel = r + pad - c
    kpos = jnp.arange(nb)[:, None, None] * Q_BLOCK - pad + c[None]
    mask = (rel >= 0)[None] & (rel < window)[None] & (kpos >= 0)
    s = jnp.einsum('bnqhgd,bnchd->bnhgqc', qb, kb).astype(jnp.float32) * (D ** -0.5)
    s = jnp.where(mask[None, :, None, None], s, NEG_INF)
    if sinks is not None:
        sink = jnp.broadcast_to(sinks.astype(jnp.float32).reshape(1, 1, Hkv, G, 1, 1), s.shape[:-1] + (1,))
        p = jax.nn.softmax(jnp.concatenate([s, sink], axis=-1), axis=-1)[..., :-1]
    else:
        p = jax.nn.softmax(s, axis=-1)
    o = jnp.einsum('bnhgqc,bnchd->bnqhgd', p.astype(v.dtype), vb)
    return o.reshape(B, T, H, D)


def dsa_attention(q, k, v, iq, ik, iw):
    B, T, H, D = q.shape
    Hkv = k.shape[2]
    G = H // Hkv
    k_top = min(DSA_TOPK_MAX, T // 4)
    nb = T // Q_BLOCK
    kpos = jnp.arange(T)
    gather = jax.vmap(lambda table, idx: table[idx])

    def one_block(inp):
        i, qb, iqb, iwb = inp
        qpos = i * Q_BLOCK + jnp.arange(Q_BLOCK)
        rel = jax.nn.relu(jnp.einsum('bqhd,bsd->bqhs', iqb, ik).astype(jnp.float32) * (IDX_DIM ** -0.5))
        score = jnp.einsum('bqh,bqhs->bqs', iwb.astype(jnp.float32), rel)
        score = jnp.where((kpos[None, :] <= qpos[:, None])[None], score, NEG_INF)
        _, idx = lax.top_k(score, k_top)
        kg, vg = gather(k, idx), gather(v, idx)
        s = jnp.einsum('bqhgd,bqkhd->bqhgk', qb.reshape(B, Q_BLOCK, Hkv, G, D), kg).astype(jnp.float32) * (D ** -0.5)
        p = masked_softmax(s, (idx <= qpos[None, :, None])[:, :, None, None, :])
        o = jnp.einsum('bqhgk,bqkhd->bqhgd', p.astype(v.dtype), vg)
        return o.reshape(B, Q_BLOCK, H, D)

    out = lax.map(one_block, (jnp.arange(nb), to_blocks(q, nb), to_blocks(iq, nb), to_blocks(iw, nb)))
    return from_blocks(out)


def nsa_attention(q, kc, vc, ks, vs, kw, vw, gates, pe_k, pe_v, w_ck1, w_ck2, w_cv1, w_cv2):
    B, T, H, D = q.shape
    Hkv = kc.shape[2]
    G = H // Hkv
    scale = D ** -0.5
    tpos = jnp.arange(T)
    qg = q.reshape(B, T, Hkv, G, D)

    n_cmp = (T - CMP_LEN) // CMP_STRIDE + 1
    cstart = jnp.arange(n_cmp) * CMP_STRIDE
    win = cstart[:, None] + jnp.arange(CMP_LEN)[None, :]

    def compress(t, pe, w1, w2):
        blk = t[:, win] + pe[None, None, :, None, :]
        blk = jnp.swapaxes(blk, 2, 3).reshape(B, n_cmp, Hkv, CMP_LEN * D)
        return jax.nn.relu(blk @ w1) @ w2

    k_cmp = compress(kc, pe_k, w_ck1, w_ck2)
    v_cmp = compress(vc, pe_v, w_cv1, w_cv2)
    cmask = (cstart + CMP_LEN - 1)[None, :] <= tpos[:, None]
    s_cmp = jnp.einsum('bthgd,bchd->bthgc', qg, k_cmp).astype(jnp.float32) * scale
    p_cmp = masked_softmax(s_cmp, cmask[None, :, None, None, :])
    o_cmp = jnp.einsum('bthgc,bchd->bthgd', p_cmp.astype(vc.dtype), v_cmp)

    n_sel = T // SLC_LEN
    n_top = min(SLC_TOPN, n_sel)
    sstart = jnp.arange(n_sel) * SLC_LEN
    overlap = ((cstart[:, None] < sstart[None, :] + SLC_LEN)
               & (cstart[:, None] + CMP_LEN > sstart[None, :])).astype(jnp.float32)
    imp = jnp.einsum('bthgc,cj->bthj', p_cmp, overlap)
    cur = (tpos // SLC_LEN)[:, None]
    jb = jnp.arange(n_sel)[None, :]
    forced = (jb == 0) | (jb == cur) | (jb == cur - 1)
    imp = jnp.where(forced[None, :, None, :], FORCE_SCORE, imp)
    imp = jnp.where((sstart[None, :] <= tpos[:, None])[None, :, None, :], imp, NEG_INF)
    _, sel = lax.top_k(imp, n_top)
    ks_blk = jnp.moveaxis(ks.reshape(B, n_sel, SLC_LEN, Hkv, D), 3, 1)
    vs_blk = jnp.moveaxis(vs.reshape(B, n_sel, SLC_LEN, Hkv, D), 3, 1)
    gather = jax.vmap(jax.vmap(lambda table, idx: table[idx]))
    n_qb = T // SLC_Q_BLOCK
    n_keys = n_top * SLC_LEN

    def one_block(inp):
        i, qb, selb = inp
        qpos = i * SLC_Q_BLOCK + jnp.arange(SLC_Q_BLOCK)
        st = jnp.swapaxes(selb, 1, 2)
        kg = gather(ks_blk, st).reshape(B, Hkv, SLC_Q_BLOCK, n_keys, D)
        vg = gather(vs_blk, st).reshape(B, Hkv, SLC_Q_BLOCK, n_keys, D)
        kpos = (st[..., None] * SLC_LEN + jnp.arange(SLC_LEN)).reshape(B, Hkv, SLC_Q_BLOCK, n_keys)
        valid = jnp.swapaxes(kpos <= qpos[None, None, :, None], 1, 2)
        s = jnp.einsum('bqhgd,bhqkd->bqhgk', qb, kg).astype(jnp.float32) * scale
        p = masked_softmax(s, valid[:, :, :, None, :])
        return jnp.einsum('bqhgk,bhqkd->bqhgd', p.astype(vs.dtype), vg)

    o_slc = from_blocks(lax.map(one_block, (jnp.arange(n_qb), to_blocks(qg, n_qb), to_blocks(sel, n_qb))))

    o_win = banded_attention(q, kw, vw, NSA_WINDOW, None)

    g = jax.nn.sigmoid(gates.reshape(B, T, H, 3))
    return (g[..., 0:1] * o_cmp.reshape(B, T, H, D)
            + g[..., 1:2] * o_slc.reshape(B, T, H, D)
            + g[..., 2:3] * o_win)


def setup_inputs(seed: int = 0) -> dict:
    key = jax.random.key(seed)
    ks = jax.random.split(key, 18)

    def nrm(k, shape, scale):
        return jax.random.normal(k, shape, jnp.float32) * scale

    return {
        "x": nrm(ks[0], (BATCH, SEQ, D_MODEL), 1.0),
        "norm_mix": 1.0 + nrm(ks[1], (DEPTH, D_MODEL), 0.1),
        "w_in": nrm(ks[2], (DEPTH, D_MODEL, D_IN), D_MODEL ** -0.5),
        "q_norm": 1.0 + nrm(ks[3], (DEPTH, N_MIXERS, HEAD_DIM), 0.1),
        "k_norm": 1.0 + nrm(ks[4], (DEPTH, N_MIXERS, HEAD_DIM), 0.1),
        "sinks": nrm(ks[5], (DEPTH, N_Q_HEADS), 1.0),
        "cmp_pe_k": nrm(ks[6], (DEPTH, CMP_LEN, HEAD_DIM), 0.1),
        "cmp_pe_v": nrm(ks[7], (DEPTH, CMP_LEN, HEAD_DIM), 0.1),
        "w_ck1": nrm(ks[8], (DEPTH, CMP_LEN * HEAD_DIM, CMP_HIDDEN), (CMP_LEN * HEAD_DIM) ** -0.5),
        "w_ck2": nrm(ks[9], (DEPTH, CMP_HIDDEN, HEAD_DIM), (CMP_HIDDEN / 2) ** -0.5),
        "w_cv1": nrm(ks[10], (DEPTH, CMP_LEN * HEAD_DIM, CMP_HIDDEN), (CMP_LEN * HEAD_DIM) ** -0.5),
        "w_cv2": nrm(ks[11], (DEPTH, CMP_HIDDEN, HEAD_DIM), (CMP_HIDDEN / 2) ** -0.5),
        "w_branch": nrm(ks[12], (DEPTH, N_MIXERS, MIX_WIDTH, D_MODEL), MIX_WIDTH ** -0.5),
        "w_out": nrm(ks[13], (DEPTH, D_MODEL, D_MODEL), D_MODEL ** -0.5),
        "norm_mlp": 1.0 + nrm(ks[14], (DEPTH, D_MODEL), 0.1),
        "w_up": nrm(ks[15], (DEPTH, D_MODEL, D_FF), D_MODEL ** -0.5),
        "w_down": nrm(ks[16], (DEPTH, D_FF, D_MODEL), D_FF ** -0.5),
    }


def reference(x, norm_mix, w_in, q_norm, k_norm, sinks, cmp_pe_k, cmp_pe_v,
              w_ck1, w_ck2, w_cv1, w_cv2, w_branch, w_out, norm_mlp, w_up, w_down):
    B, T, _ = x.shape
    cos, sin = rope_tables(T, HEAD_DIM)

    def heads(t, n):
        return t.reshape(B, T, n, -1)

    def qk(t, n, gain):
        return apply_rope(rms_norm(heads(t, n), gain), cos, sin)

    for l in range(DEPTH):
        h = rms_norm(x, norm_mix[l])
        z = h @ w_in[l]
        (qa, ka, va, iq, ik, iw, qb, kb, vb,
         qc, kc, vc, ksl, vsl, kwn, vwn, gc, gm) = jnp.split(z, IN_OFFSETS, axis=-1)

        o_a = dsa_attention(qk(qa, N_Q_HEADS, q_norm[l, 0]), qk(ka, N_KV_HEADS, k_norm[l, 0]),
                            heads(va, N_KV_HEADS),
                            apply_rope(heads(iq, IDX_HEADS), cos, sin),
                            apply_rope(heads(ik, 1), cos, sin)[:, :, 0],
                            iw * (IDX_HEADS ** -0.5))
        o_b = banded_attention(qk(qb, N_Q_HEADS, q_norm[l, 1]), qk(kb, N_KV_HEADS, k_norm[l, 1]),
                               heads(vb, N_KV_HEADS), SWA_WINDOW, sinks[l])
        kn = k_norm[l, 2]
        o_c = nsa_attention(qk(qc, N_Q_HEADS, q_norm[l, 2]),
                            qk(kc, N_KV_HEADS, kn), heads(vc, N_KV_HEADS),
                            qk(ksl, N_KV_HEADS, kn), heads(vsl, N_KV_HEADS),
                            qk(kwn, N_KV_HEADS, kn), heads(vwn, N_KV_HEADS),
                            gc, cmp_pe_k[l], cmp_pe_v[l], w_ck1[l], w_ck2[l], w_cv1[l], w_cv2[l])

        o_all = jnp.stack([o_a, o_b, o_c], axis=2).reshape(B, T, N_MIXERS, MIX_WIDTH)
        y = jnp.einsum('btnc,ncd->btnd', o_all, w_branch[l])
        g = jax.nn.sigmoid(gm.reshape(B, T, N_MIXERS, D_MODEL))
        x = x + jnp.sum(g * y, axis=2) @ w_out[l]

        h2 = rms_norm(x, norm_mlp[l])
        x = x + jnp.square(jax.nn.relu(h2 @ w_up[l])) @ w_down[l]
    return x
```

```python
import numpy as np
from contextlib import ExitStack
import concourse.bass as bass
import concourse.mybir as mybir
from concourse.bass_utils import run_bass_kernel_spmd
import ml_dtypes

F32 = mybir.dt.float32
BF16 = mybir.dt.bfloat16
ALU = mybir.AluOpType
AF = mybir.ActivationFunctionType
AX = mybir.AxisListType

T = 4096
D = 1024
NT = T // 128
DEPTH = 2
D_IN = 6236
D_FF = 4096
EPS = 1e-6
NEG = -1e30
N_CORES = 8

OFF = dict(qa=0, ka=512, va=640, iq=768, ik=1024, iw=1088, qb=1092, kb=1604, vb=1732,
           qc=1860, kc=2372, vc=2500, ks=2628, vs=2756, kw=2884, vw=3012, gc=3140, gm=3164)


DBG = {}
_UNIQ = [0]


def _uniq(name):
    _UNIQ[0] += 1
    return "%s_%d" % (name, _UNIQ[0])


class Buf:
    __slots__ = ("w", "r", "dsi", "excl")

    def __init__(self, excl=False):
        self.w = None
        self.r = {}
        self.dsi = None
        self.excl = excl


class Eng:
    def __init__(self, name, h, si, selfsync):
        self.name, self.h, self.si, self.selfsync = name, h, si, selfsync
        self.seen = {}


class KB:
    def __init__(self, nc, es, n_dma_sems=72):
        self.nc = nc
        self.sems = []
        self.semval = []
        self.engs = {}
        for name, h, ss in [("pe", nc.tensor, False), ("act", nc.scalar, True), ("dve", nc.vector, True),
                            ("pool", nc.gpsimd, True), ("sp", nc.sync, False)]:
            sem = es.enter_context(nc.semaphore("sem_" + name))
            self.sems.append(sem)
            self.semval.append(0)
            self.engs[name] = Eng(name, h, len(self.sems) - 1, ss)
        self.bar_si = len(self.sems)
        self.sems.append(es.enter_context(nc.semaphore("sem_bar")))
        self.semval.append(0)
        self.dma_free = []
        self.dma_free_sw = []
        for i in range(n_dma_sems):
            self.sems.append(es.enter_context(nc.semaphore("dsem%d" % i)))
            self.semval.append(0)
            (self.dma_free_sw if i < 8 else self.dma_free).append(len(self.sems) - 1)
        self.sw_sems = set(self.dma_free_sw)
        self.phase_bufs = []
        self.n_inst = 0

    def buf(self, dma=False, excl=False):
        b = Buf(excl)
        if dma:
            b.dsi = (self.dma_free_sw if dma == "sw" else self.dma_free).pop()
            self.phase_bufs.append(b)
        return b

    def bufs(self, n, dma=False, excl=False):
        return [self.buf(dma, excl) for _ in range(n)]

    @staticmethod
    def _split(reads, writes):
        ex = [b for b in reads if b.excl]
        if ex:
            return [b for b in reads if not b.excl], list(writes) + ex
        return reads, writes

    def _deps(self, reads, writes):
        deps = {}
        for b in reads:
            if b.w is not None:
                si, v = b.w
                if deps.get(si, 0) < v:
                    deps[si] = v
        for b in writes:
            if b.w is not None:
                si, v = b.w
                if deps.get(si, 0) < v:
                    deps[si] = v
            for si, v in b.r.items():
                if deps.get(si, 0) < v:
                    deps[si] = v
        return deps

    def _wait(self, e, deps):
        for si, v in deps.items():
            if e.seen.get(si, 0) >= v:
                continue
            if si == e.si and not e.selfsync:
                continue
            e.h.wait_ge(self.sems[si], v)
            e.seen[si] = v
            self.n_inst += 1

    def _mark(self, ev, reads, writes):
        si, v = ev
        for b in reads:
            b.r[si] = v
        for b in writes:
            b.w = ev
            b.r = {}

    def op(self, ename, fn, reads=(), writes=()):
        reads, writes = self._split(reads, writes)
        e = self.engs[ename]
        self._wait(e, self._deps(reads, writes))
        ins = fn(e.h)
        self.semval[e.si] += 1
        ins.then_inc(self.sems[e.si], 1)
        self.n_inst += 1
        self._mark((e.si, self.semval[e.si]), reads, writes)

    def dma(self, qname, out, in_, reads=(), writes=(), **kw):
        e = self.engs[qname]
        self._wait(e, self._deps(reads, writes))
        b0 = None
        for b in list(writes) + list(reads):
            if b.dsi is not None:
                b0 = b
                break
        assert b0 is not None
        assert (b0.dsi in self.sw_sems) == (qname == "pool"), "DMA queue / semaphore class mismatch"
        ins = e.h.dma_start(out=out, in_=in_, **kw)
        self.semval[b0.dsi] += 16
        ins.then_inc(self.sems[b0.dsi], 16)
        self.n_inst += 1
        self._mark((b0.dsi, self.semval[b0.dsi]), reads, writes)

    def barrier(self):
        sp = self.engs["sp"]
        for si in range(len(self.sems)):
            if si == self.bar_si or si == sp.si:
                continue
            v = self.semval[si]
            if v > 0 and sp.seen.get(si, 0) < v:
                sp.h.wait_ge(self.sems[si], v)
                sp.seen[si] = v
        self.semval[self.bar_si] += 1
        sp.h.sem_inc(self.sems[self.bar_si], 1)
        bv = self.semval[self.bar_si]
        for name, e in self.engs.items():
            if name != "sp":
                e.h.wait_ge(self.sems[self.bar_si], bv)
            for si in range(len(self.sems)):
                e.seen[si] = self.semval[si]
        for b in self.phase_bufs:
            (self.dma_free_sw if b.dsi in self.sw_sems else self.dma_free).append(b.dsi)
            b.dsi = None
        self.phase_bufs = []


def _rope_tables():
    inv = np.power(np.float32(10000.0), -(np.arange(0, 64, 2, dtype=np.float32) / np.float32(64))).astype(np.float32)
    ang = (np.arange(T, dtype=np.float32)[:, None] * inv[None, :]).astype(np.float32)
    cos = np.cos(ang).astype(np.float32)
    sin = np.sin(ang).astype(np.float32)
    cosT = np.zeros((128, T), np.float32)
    sinT = np.zeros((128, T), np.float32)
    for p in range(128):
        d = p % 64
        cosT[p] = cos[:, d % 32]
        sinT[p] = sin[:, d % 32] * (-1.0 if d < 32 else 1.0)
    return cosT, sinT


def _consts():
    c = {}
    bf = ml_dtypes.bfloat16
    c["c_ident"] = np.eye(128, dtype=np.float32).astype(bf)
    cosT, sinT = _rope_tables()
    c["c_cos"] = cosT
    c["c_sin"] = sinT
    P = np.zeros((128, 128), np.float32)
    for m in range(128):
        d = m % 64
        base = m - d
        if d < 32:
            P[base + d + 32, m] = 1.0
        else:
            P[base + d - 32, m] = 1.0
    c["c_rot"] = P
    B1 = np.zeros((128, 128), np.float32)
    B1[:64, :64] = 1.0
    B1[64:, 64:] = 1.0
    c["c_bones"] = B1.astype(bf)
    k = np.arange(128)[:, None]
    q = np.arange(128)[None, :]
    c["c_mdiag"] = (k <= q).astype(np.float32).astype(bf)
    c["c_mprev"] = (k > q).astype(np.float32).astype(bf)
    c["c_mdiagb"] = np.where(k <= q, 0.0, -30000.0).astype(np.float32).astype(bf)
    c["c_mprevb"] = np.where(k > q, 0.0, -30000.0).astype(np.float32).astype(bf)
    c["c_negdiag"] = np.where(q.T >= k.T, 0.0, NEG).astype(np.float32)
    qq = np.arange(128)[:, None]
    kk = np.arange(128)[None, :]
    c["c_negdiag"] = np.where(kk <= qq, 0.0, NEG).astype(np.float32)
    cidx = np.arange(256)
    tpos = np.arange(T)
    cm = ((16 * cidx[:, None] + 31) <= tpos[None, :]) & (cidx[:, None] < 255)
    c["c_mcmp"] = cm.reshape(2, 128, T).transpose(1, 0, 2).astype(np.float32).astype(bf).copy()
    j = np.arange(64)
    ov = ((16 * cidx[:, None] < 64 * j[None, :] + 64) & (16 * cidx[:, None] + 32 > 64 * j[None, :]) & (cidx[:, None] < 255))
    c["c_ovl"] = ov.reshape(2, 128, 64).transpose(1, 0, 2).astype(np.float32).astype(bf).copy()
    c["c_bsel"] = (np.arange(T)[None, :] // 64 == j[:, None]).astype(np.float32).astype(bf)
    cur = tpos // 64
    forced = (j[None, :] == 0) | (j[None, :] == cur[:, None]) | (j[None, :] == cur[:, None] - 1)
    noncausal = (64 * j[None, :]) > tpos[:, None]
    F1 = np.where(forced, 1e9, 0.0).astype(np.float32)
    F2 = np.where(noncausal, NEG, 3e38).astype(np.float32)
    gs = np.zeros((32, 24, 64), np.float32)
    for r_ in range(24):
        gs[r_, r_, :] = 1.0
    c["c_gsel"] = gs
    c["c_p2"] = np.tile((0.5 ** np.arange(1, 25, dtype=np.float64)).astype(np.float32)[None, :], (128, 1))
    c["c_f1"] = F1.reshape(NT, 128, 64).transpose(1, 0, 2).copy()
    c["c_f2"] = F2.reshape(NT, 128, 64).transpose(1, 0, 2).copy()
    return c


CONST_SPECS = None


def build_program(depth=DEPTH, debug=False, stop_after=None):
    nc = bass.Bass("TRN2", target_bir_lowering=False)
    consts = _consts()

    def dram_in(name, shape, dt=F32):
        return nc.dram_tensor(name, list(shape), dt, kind="ExternalInput").ap()

    scratch_kind = "ExternalOutput" if debug else "Internal"

    def dram_scr(name, shape, dt):
        return nc.dram_tensor(name, list(shape), dt, kind=scratch_kind).ap()

    x_in = dram_in("x", [T, D])
    norm_mix = dram_in("norm_mix", [DEPTH, D])
    w_in = dram_in("w_in", [DEPTH, D, D_IN])
    gains = dram_in("gains", [DEPTH, 128, 6])
    sinks = dram_in("sinks", [DEPTH, 8])
    peT = dram_in("peT", [DEPTH, 2, 128, 32])
    w_ck1 = dram_in("w_ck1", [DEPTH, 2048, 256])
    w_ck2 = dram_in("w_ck2", [DEPTH, 256, 64])
    w_cv1 = dram_in("w_cv1", [DEPTH, 2048, 256])
    w_cv2 = dram_in("w_cv2", [DEPTH, 256, 64])
    w_branch = dram_in("w_branch", [DEPTH, 3, 512, D])
    w_out = dram_in("w_out", [DEPTH, D, D])
    norm_mlp = dram_in("norm_mlp", [DEPTH, D])
    w_up = dram_in("w_up", [DEPTH, D, D_FF])
    w_down = dram_in("w_down", [DEPTH, D_FF, D])
    cin = {}
    for k, v in consts.items():
        cin[k] = dram_in(k, v.shape, BF16 if v.dtype == ml_dtypes.bfloat16 else F32)
    y_out = nc.dram_tensor("y", [T, D], F32, kind="ExternalOutput").ap()

    xs = [dram_scr("xs0", [T, D], F32), dram_scr("xs1", [T, D], F32)]
    QT = [dram_scr("QT%d" % m, [128, 4, T], BF16) for m in range(3)]
    KT = [dram_scr("KT%d" % m, [128, T], BF16) for m in range(5)]
    VcT = dram_scr("VcT", [128, T], BF16)
    IqT = dram_scr("IqT", [128, 2, T], BF16)
    IkT = dram_scr("IkT", [128, T], BF16)
    sgT = dram_scr("sgT", [128, 24, T], BF16)
    Vtok = dram_scr("Vtok", [T, 512], BF16)
    gciw = dram_scr("gciw", [T, 32], F32)
    OT = [dram_scr("OT%d" % m, [64, 8, T], BF16) for m in range(3)]
    KcmpT = dram_scr("KcmpT", [128, 256], BF16)
    Vcmp = dram_scr("Vcmp", [128, 2, 2, 64], BF16)
    sgcT2 = dram_scr("sgcT2", [32, T], F32)

    with ExitStack() as es:
        kb = KB(nc, es)
        for l in range(depth):
            x_src = x_in if l == 0 else xs[1]
            x_dst = y_out if l == depth - 1 else xs[1]
            phase_proj(nc, kb, l, x_src, norm_mix, w_in, gains, cin, QT, KT, VcT, IqT, IkT, sgT, Vtok, gciw, sgcT2, stop=stop_after)
            kb.barrier()
            if stop_after in ("proj", "norm"):
                break
            if DBG.get("skip_a") is None:
                phase_dsa(nc, kb, l, cin, QT[0], KT[0], IqT, IkT, Vtok, gciw, OT[0], qtiles=DBG.get("qtiles"))
            if stop_after == "dsa":
                break
            phase_swa(nc, kb, l, cin, sinks, QT[1], KT[1], Vtok, OT[1], qtiles=DBG.get("qtiles"))
            if stop_after == "swa":
                break
            phase_cmp(nc, kb, l, KT[2], VcT, peT, w_ck1, w_ck2, w_cv1, w_cv2, KcmpT, Vcmp)
            phase_nsa(nc, kb, l, cin, QT[2], KT[3], KT[4], Vtok, KcmpT, Vcmp, sgcT2, OT[2], qtiles=DBG.get("qtiles"))
            if stop_after == "nsa":
                break
            phase_merge(nc, kb, l, x_src, xs[0], OT, sgT, w_branch, w_out)
            if stop_after == "merge":
                break
            phase_mlp(nc, kb, l, xs[0], x_dst, norm_mlp, w_up, w_down, cin)
        kb.barrier()
    return nc, consts


def phase_proj(nc, kb, l, x_src, norm_mix, w_in, gains, cin, QT, KT, VcT, IqT, IkT, sgT, Vtok, gciw, sgcT2, stop=None):
    with ExitStack() as es:
        def sb(name, shape, dt):
            return es.enter_context(nc.sbuf_tensor(_uniq(name), list(shape), dt))

        def ps(name, shape, dt):
            return es.enter_context(nc.psum_tensor(_uniq(name), list(shape), dt))

        hT = sb("hT", [128, 8, T], BF16)
        hT_b = kb.bufs(8)
        gt = sb("gt", [128, D], F32)
        gt_b = kb.buf(dma=True)
        ident = sb("ident", [128, 128], BF16)
        ident_b = kb.buf(dma=True)
        kb.dma("sp", gt[:], norm_mix[l].partition_broadcast(128), writes=[gt_b])
        kb.dma("sp", ident[:], cin["c_ident"], writes=[ident_b])

        xt = [sb("xt%d" % i, [128, D], F32) for i in range(2)]
        xt_b = kb.bufs(2, dma=True)
        junk = sb("junk", [128, D], F32)
        junk_b = kb.buf()
        st = [sb("st%d" % i, [128, 4], F32) for i in range(2)]
        st_b = kb.bufs(2)
        hb = [sb("hb%d" % i, [128, D], BF16) for i in range(2)]
        hb_b = kb.bufs(2)
        pst = [ps("pst%d" % i, [128, 8, 128], BF16) for i in range(2)]
        pst_b = kb.bufs(2, excl=True)
        for tt in range(NT):
            i = tt % 2
            kb.dma("sp", xt[i][:], x_src[tt * 128:(tt + 1) * 128, :], writes=[xt_b[i]])
            kb.op("act", lambda e: e.activation(out=junk[:], in_=xt[i][:], func=AF.Square, accum_out=st[i][:, 0:1]),
                  reads=[xt_b[i]], writes=[junk_b, st_b[i]])
            kb.op("act", lambda e: e.activation(out=st[i][:, 1:2], in_=st[i][:, 0:1], func=AF.Sqrt, bias=EPS, scale=1.0 / D),
                  reads=[st_b[i]], writes=[st_b[i]])
            kb.op("dve", lambda e: e.reciprocal(out=st[i][:, 2:3], in_=st[i][:, 1:2]), reads=[st_b[i]], writes=[st_b[i]])
            kb.op("dve", lambda e: e.scalar_tensor_tensor(out=hb[i][:], in0=xt[i][:], scalar=st[i][:, 2:3], in1=gt[:],
                                                          op0=ALU.mult, op1=ALU.mult),
                  reads=[xt_b[i], st_b[i], gt_b], writes=[hb_b[i]])
            for kc in range(8):
                kb.op("pe", lambda e: e.transpose(out=pst[i][:, kc, :], in_=hb[i][:, kc * 128:(kc + 1) * 128], identity=ident[:]),
                      reads=[hb_b[i], ident_b], writes=[pst_b[i]])
            eng = "act" if tt % 2 == 0 else "pool"
            if eng == "pool":
                eng = "dve"
            kb.op(eng, lambda e: e.tensor_copy(out=hT[:, :, tt * 128:(tt + 1) * 128], in_=pst[i][:]) if eng != "act"
                  else e.activation(out=hT[:, :, tt * 128:(tt + 1) * 128], in_=pst[i][:], func=AF.Copy),
                  reads=[pst_b[i]], writes=[hT_b[tt // 4]])

        if stop == 'norm':
            dbg = sb('dbg', [128, 512], BF16); dbg_b = kb.buf(dma=True)
            kb.op('dve', lambda e: e.tensor_copy(out=dbg[:], in_=hT[:, 0, 0:512]), reads=hT_b, writes=[dbg_b])
            kb.dma('sp', KT[0][:, 0:512], dbg[:], reads=[dbg_b])
            kb.barrier()
            return
        cosT = sb("cosT", [128, T], F32)
        sinT = sb("sinT", [128, T], F32)
        cs_b = kb.buf(dma=True)
        kb.dma("sp", cosT[:], cin["c_cos"], writes=[cs_b])
        sn_b = kb.buf(dma=True)
        kb.dma("sp", sinT[:], cin["c_sin"], writes=[sn_b])
        rotc = sb("rotc", [128, 128], F32)
        rotc_b = kb.buf(dma=True)
        kb.dma("sp", rotc[:], cin["c_rot"], writes=[rotc_b])
        rot1 = sb("rot1", [128, 128], BF16)
        rot1_b = kb.buf()
        kb.op("dve", lambda e: e.tensor_copy(out=rot1[:], in_=rotc[:]), reads=[rotc_b], writes=[rot1_b])
        bones = sb("bones", [128, 128], BF16)
        bones_b = kb.buf(dma=True)
        kb.dma("sp", bones[:], cin["c_bones"], writes=[bones_b])
        gn = sb("gn", [128, 6], F32)
        gn_b = kb.buf(dma=True)
        kb.dma("sp", gn[:], gains[l], writes=[gn_b])
        rotg = sb("rotg", [128, 6, 128], BF16)
        rotg_b = kb.buf()
        for gi in range(6):
            kb.op("dve", lambda e: e.tensor_scalar(out=rotg[:, gi, :], in0=rotc[:], scalar1=gn[:, gi:gi + 1], scalar2=None, op0=ALU.mult),
                  reads=[rotc_b, gn_b], writes=[rotg_b])

        chunks = []
        for m, qn in enumerate(["qa", "qb", "qc"]):
            for g in range(4):
                chunks.append(("qk", m, QT[m][:, g, :], [(0, OFF[qn] + g * 64, 64), (64, OFF[qn] + (4 + g) * 64, 64)]))
        for ki, (kn, m) in enumerate([("ka", 0), ("kb", 1), ("kc", 2), ("ks", 2), ("kw", 2)]):
            chunks.append(("qk", 3 + m, KT[ki], [(0, OFF[kn], 128)]))
        chunks.append(("copy", None, VcT, [(0, OFF["vc"], 128)]))
        for c in range(2):
            chunks.append(("rope", None, IqT[:, c, :], [(0, OFF["iq"] + c * 128, 128)]))
        chunks.append(("rope", None, IkT, [(0, OFF["ik"], 64), (64, OFF["ik"], 64)]))
        for c in range(24):
            chunks.append(("sig", None, sgT[:, c, :], [(0, OFF["gm"] + c * 128, 128)]))
        chunks.append(("sigf", None, sgcT2, [(0, OFF["gc"], 24)]))

        wc = [sb("wc%d" % i, [128, 8, 128], BF16) for i in range(2)]
        wc_b = kb.bufs(2, dma="sw")
        psz = [ps("psz%d" % i, [128, 512], F32) for i in range(2)]
        psz_b = kb.bufs(2, excl=True)
        pss = ps("pss", [128, 512], F32)
        pss_b = kb.buf(excl=True)
        psr = ps("psr", [128, 512], F32)
        psr_b = kb.buf(excl=True)
        sq = sb("sq", [128, 512], BF16); sq_b = kb.buf()
        zb = sb("zb", [128, 512], BF16); zb_b = kb.buf()
        rs = sb("rs", [128, 512], F32); rs_b = kb.buf()
        rinv = sb("rinv", [128, 512], F32); rinv_b = kb.buf()
        t1 = sb("t1", [128, 512], F32); t1_b = kb.buf()
        t2 = sb("t2", [128, 512], F32); t2_b = kb.buf()
        t3 = sb("t3", [128, 512], F32); t3_b = kb.buf()
        og = [sb("og%d" % i, [128, 512], BF16) for i in range(2)]
        og_b = kb.bufs(2, dma=True)
        ogf = sb("ogf", [32, 512], F32)
        ogf_b = kb.buf(dma=True)
        w_in_l = w_in[l]
        blk = 0
        if DBG.get('chunks') is not None:
            chunks = [chunks[i] for i in DBG['chunks']]
        sq2 = [sq, sb("sq_b", [128, 512], BF16)]; sq2_b = [sq_b, kb.buf()]
        zb2 = [zb, sb("zb_b", [128, 512], BF16)]; zb2_b = [zb_b, kb.buf()]
        ppipe = Pipe(1)
        for ci, (kind, gi, dst, pieces) in enumerate(chunks):
            wi = ci % 2
            for (poff, c0, n) in pieces:
                kb.dma("pool", wc[wi][:, :, poff:poff + n],
                       w_in_l[:, c0:c0 + n].rearrange("(kc p) n -> p kc n", p=128), writes=[wc_b[wi]])
            for t4 in range(8):
                zi = blk % 2
                oi = blk % 2
                blk += 1
                tsl = slice(t4 * 512, (t4 + 1) * 512)

                def front(kind=kind, gi=gi, wi=wi, t4=t4, zi=zi, tsl=tsl):
                    for kc in range(8):
                        kb.op("pe", lambda e: e.matmul(psz[zi][:], lhsT=wc[wi][:, kc, :], rhs=hT[:, kc, tsl], start=(kc == 0), stop=(kc == 7)),
                              reads=[wc_b[wi], hT_b[t4]], writes=[psz_b[zi]])
                    if kind in ("qk", "rope"):
                        kb.op("act", lambda e: e.activation(out=zb2[zi][:], in_=psz[zi][:], func=AF.Copy), reads=[psz_b[zi]], writes=[zb2_b[zi]])
                    if kind == "qk":
                        kb.op("act", lambda e: e.activation(out=sq2[zi][:], in_=psz[zi][:], func=AF.Square), reads=[psz_b[zi]], writes=[sq2_b[zi]])

                def back(kind=kind, gi=gi, dst=dst, zi=zi, oi=oi, tsl=tsl):
                    zb, zb_b, sq, sq_b = zb2[zi], zb2_b[zi], sq2[zi], sq2_b[zi]
                    if kind == "sig":
                        kb.op("act", lambda e: e.activation(out=og[oi][:], in_=psz[zi][:], func=AF.Sigmoid), reads=[psz_b[zi]], writes=[og_b[oi]])
                    elif kind == "sigf":
                        kb.op("act", lambda e: e.activation(out=ogf[:], in_=psz[zi][0:32, :], func=AF.Sigmoid), reads=[psz_b[zi]], writes=[ogf_b])
                        kb.dma("sp", dst[:, tsl], ogf[:], reads=[ogf_b])
                        return
                    elif kind == "copy":
                        kb.op("act", lambda e: e.activation(out=og[oi][:], in_=psz[zi][:], func=AF.Copy), reads=[psz_b[zi]], writes=[og_b[oi]])
                    else:
                        if kind == "qk":
                            kb.op("pe", lambda e: e.matmul(pss[:], lhsT=bones[:], rhs=sq[:], start=True, stop=True),
                                  reads=[bones_b, sq_b], writes=[pss_b])
                            kb.op("pe", lambda e: e.matmul(psr[:], lhsT=rotg[:, gi, :], rhs=zb[:], start=True, stop=True),
                                  reads=[rotg_b, zb_b], writes=[psr_b])
                            kb.op("act", lambda e: e.activation(out=rs[:], in_=pss[:], func=AF.Ln, bias=EPS, scale=1.0 / 64),
                                  reads=[pss_b], writes=[rs_b])
                            kb.op("act", lambda e: e.activation(out=rinv[:], in_=rs[:], func=AF.Exp, scale=-0.5), reads=[rs_b], writes=[rinv_b])
                            kb.op("dve", lambda e: e.scalar_tensor_tensor(out=t1[:], in0=psz[zi][:], scalar=gn[:, gi:gi + 1], in1=cosT[:, tsl],
                                                                          op0=ALU.mult, op1=ALU.mult),
                                  reads=[psz_b[zi], gn_b, cs_b], writes=[t1_b])
                        else:
                            kb.op("pe", lambda e: e.matmul(psr[:], lhsT=rot1[:], rhs=zb[:], start=True, stop=True),
                                  reads=[rot1_b, zb_b], writes=[psr_b])
                            kb.op("dve", lambda e: e.tensor_tensor(out=t1[:], in0=psz[zi][:], in1=cosT[:, tsl], op=ALU.mult),
                                  reads=[psz_b[zi], cs_b], writes=[t1_b])
                        kb.op("dve", lambda e: e.tensor_tensor(out=t2[:], in0=psr[:], in1=sinT[:, tsl], op=ALU.mult),
                              reads=[psr_b, sn_b], writes=[t2_b])
                        if kind == "qk":
                            kb.op("pool", lambda e: e.tensor_tensor(out=t3[:], in0=t1[:], in1=t2[:], op=ALU.add), reads=[t1_b, t2_b], writes=[t3_b])
                            kb.op("pool", lambda e: e.tensor_tensor(out=og[oi][:], in0=t3[:], in1=rinv[:], op=ALU.mult),
                                  reads=[t3_b, rinv_b], writes=[og_b[oi]])
                        else:
                            kb.op("pool", lambda e: e.tensor_tensor(out=og[oi][:], in0=t1[:], in1=t2[:], op=ALU.add), reads=[t1_b, t2_b], writes=[og_b[oi]])
                    kb.dma("sp", dst[:, tsl], og[oi][:], reads=[og_b[oi]])

                ppipe.push(front, back)
        ppipe.flush()

        if DBG.get('notok'):
            kb.barrier()
            return
        wv = sb("wv", [128, 8, 512], BF16)
        wv_b = kb.buf(dma="sw")
        for vi, vn in enumerate(["va", "vb", "vs", "vw"]):
            kb.dma("pool", wv[:, :, vi * 128:(vi + 1) * 128],
                   w_in_l[:, OFF[vn]:OFF[vn] + 128].rearrange("(kc p) n -> p kc n", p=128), writes=[wv_b])
        wg = sb("wg", [128, 8, 32], BF16)
        wg_b = kb.buf(dma="sw")
        kb.op("dve", lambda e: e.memset(wg[:], 0.0), writes=[wg_b])
        kb.dma("pool", wg[:, :, 0:24], w_in_l[:, OFF["gc"]:OFF["gc"] + 24].rearrange("(kc p) n -> p kc n", p=128), writes=[wg_b])
        kb.dma("pool", wg[:, :, 24:28], w_in_l[:, OFF["iw"]:OFF["iw"] + 4].rearrange("(kc p) n -> p kc n", p=128), writes=[wg_b])
        psg = [ps("psg%d" % i, [128, 512], F32) for i in range(2)]
        psg_b = kb.bufs(2, excl=True)
        ov = [sb("ov%d" % i, [128, 512], BF16) for i in range(2)]
        ov_b = kb.bufs(2, dma=True)
        ogc = [sb("ogc%d" % i, [128, 32], F32) for i in range(2)]
        ogc_b = kb.bufs(2, dma=True)
        for tt in range(NT):
            i = tt % 2
            tsl = slice(tt * 128, (tt + 1) * 128)
            for kc in range(8):
                kb.op("pe", lambda e: e.matmul(psz[i][:], lhsT=hT[:, kc, tsl], rhs=wv[:, kc, :], start=(kc == 0), stop=(kc == 7)),
                      reads=[wv_b, hT_b[tt // 4]], writes=[psz_b[i]])
            for kc in range(8):
                kb.op("pe", lambda e: e.matmul(psg[i][:, 0:32], lhsT=hT[:, kc, tsl], rhs=wg[:, kc, :], start=(kc == 0), stop=(kc == 7)),
                      reads=[wg_b, hT_b[tt // 4]], writes=[psg_b[i]])
            kb.op("act", lambda e: e.activation(out=ov[i][:], in_=psz[i][:], func=AF.Copy), reads=[psz_b[i]], writes=[ov_b[i]])
            kb.op("dve", lambda e: e.tensor_copy(out=ogc[i][:], in_=psg[i][:, 0:32]), reads=[psg_b[i]], writes=[ogc_b[i]])
            kb.dma("sp", Vtok[tsl, :], ov[i][:], reads=[ov_b[i]])
            kb.dma("sp", gciw[tsl, :], ogc[i][:], reads=[ogc_b[i]])
        kb.barrier()


def _host_inputs(inputs, consts):
    per_core = []
    qn = np.asarray(inputs["q_norm"], np.float32)
    kn = np.asarray(inputs["k_norm"], np.float32)
    gains = np.zeros((DEPTH, 128, 6), np.float32)
    for l in range(DEPTH):
        for m in range(3):
            gains[l, :, m] = np.tile(qn[l, m], 2)
            gains[l, :, 3 + m] = np.tile(kn[l, m], 2)
    pe = np.stack([np.asarray(inputs["cmp_pe_k"], np.float32), np.asarray(inputs["cmp_pe_v"], np.float32)], axis=1)
    peT = np.ascontiguousarray(np.tile(pe.transpose(0, 1, 3, 2), (1, 1, 2, 1)))
    shared = {
        "norm_mix": np.asarray(inputs["norm_mix"], np.float32),
        "w_in": np.asarray(inputs["w_in"], np.float32),
        "gains": gains,
        "sinks": np.asarray(inputs["sinks"], np.float32),
        "peT": peT,
        "w_ck1": np.asarray(inputs["w_ck1"], np.float32),
        "w_ck2": np.asarray(inputs["w_ck2"], np.float32),
        "w_cv1": np.asarray(inputs["w_cv1"], np.float32),
        "w_cv2": np.asarray(inputs["w_cv2"], np.float32),
        "w_branch": np.asarray(inputs["w_branch"], np.float32),
        "w_out": np.asarray(inputs["w_out"], np.float32),
        "norm_mlp": np.asarray(inputs["norm_mlp"], np.float32),
        "w_up": np.asarray(inputs["w_up"], np.float32),
        "w_down": np.asarray(inputs["w_down"], np.float32),
    }
    shared.update(consts)
    x = np.asarray(inputs["x"], np.float32)
    for c in range(N_CORES):
        m = dict(shared)
        m["x"] = np.ascontiguousarray(x[c % 4])
        per_core.append(m)
    return per_core


def kernel(**inputs):
    nc, consts = build_program()
    in_maps = _host_inputs(inputs, consts)
    res = run_bass_kernel_spmd(nc, in_maps, core_ids=list(range(N_CORES)))
    out = np.stack([np.asarray(res.results[c]["y"], np.float32) for c in range(4)], axis=0)
    return out


class _Scope:
    def __init__(self, nc, es):
        self.nc, self.es = nc, es

    def sb(self, name, shape, dt):
        return self.es.enter_context(self.nc.sbuf_tensor(_uniq(name), list(shape), dt))

    def ps(self, name, shape=(128, 512), dt=F32):
        return self.es.enter_context(self.nc.psum_tensor(_uniq(name), list(shape), dt))


def _load_qkv(nc, kb, S, tag, QTd, KTd, Vtok, vi):
    q = S.sb("q_" + tag, [128, 4, T], BF16)
    q_b = kb.buf(dma=True)
    for g in range(4):
        kb.dma("sp", q[:, g, :], QTd[:, g, :], writes=[q_b])
    k = S.sb("k_" + tag, [128, T], BF16)
    k_b = kb.buf(dma=True)
    kb.dma("sp", k[:], KTd, writes=[k_b])
    v = S.sb("v_" + tag, [128, NT, 2, 65], BF16)
    v_b = kb.buf(dma=True)
    kb.op("pool", lambda e: e.memset(v[:], 1.0), writes=[v_b])
    for hk in range(2):
        kb.dma("sp", v[:, :, hk, 0:64],
               Vtok[:, vi * 128 + hk * 64: vi * 128 + hk * 64 + 64].rearrange("(kt p) d -> p kt d", p=128), writes=[v_b])
    return (q, q_b), (k, k_b), (v, v_b)


class AttnRes:
    def __init__(self, nc, kb, S, out_dt=BF16):
        self.psS = [S.ps("psS%d" % i) for i in range(2)]
        self.psS_b = kb.bufs(2, excl=True)
        self.psO = [S.ps("psO%d" % i) for i in range(2)]
        self.psO_b = kb.bufs(2, excl=True)
        self.psD = S.ps("psD")
        self.psD_b = kb.buf(excl=True)
        self.E = [S.sb("E%d" % i, [128, 512], BF16) for i in range(4)]
        self.E_b = kb.bufs(4)
        self.pipe = Pipe(DBG.get("look", 2))
        self.oTs2 = [S.sb("oTs%d" % i, [65, 512], F32) for i in range(2)]
        self.oTs2_b = kb.bufs(2)
        self.rr2 = [S.sb("rr%d" % i, [65, 512], F32) for i in range(2)]
        self.rr2_b = kb.bufs(2)
        self.oTs, self.oTs_b, self.rr, self.rr_b = self.oTs2[0], self.oTs2_b[0], self.rr2[0], self.rr2_b[0]
        self.onb = [S.sb("onb%d" % i, [64, 512], out_dt) for i in range(2)]
        self.onb_b = kb.bufs(2, dma=True)
        self.ones32 = S.sb("ones32", [65, 64], F32)
        self.ones32_b = kb.buf()
        kb.op("dve", lambda e: e.memset(self.ones32[:], 1.0), writes=[self.ones32_b])
        self.cnt = 0
        self.pcnt = 0
        self.ocnt = 0


class Pipe:
    def __init__(self, look=2):
        self.look = look
        self.pending = []
        self.bg = []
        self.quota = 0
        self.burst = 0
        self.nburst = 0

    def set_bg(self, units, nsteps, burst=0, nburst=0):
        self.bg = list(units)
        self.burst, self.nburst = burst, nburst
        rest = max(0, len(self.bg) - nburst)
        bsteps = -(-nburst // burst) if burst else 0
        self.quota = -(-rest // max(1, nsteps - bsteps))
        if DBG.get("nobg"):
            self.flush_bg()

    def push(self, front, back):
        front()
        self.pending.append(back)
        while len(self.pending) > self.look:
            self.pending.pop(0)()
        if self.nburst > 0:
            for _ in range(self.burst):
                if self.bg and self.nburst > 0:
                    self.bg.pop(0)()
                    self.nburst -= 1
            return
        for _ in range(self.quota):
            if self.bg:
                self.bg.pop(0)()

    def flush(self):
        while self.pending:
            self.pending.pop(0)()

    def flush_bg(self):
        while self.bg:
            self.bg.pop(0)()


def attn_tile(kb, R, hk, qsl, klist, K, Q, V, maskfn, extra_den=None, mask_eng=None, tiny=None, post=None, st_override=None):
    mask_eng = mask_eng or DBG.get("mask_eng", "pool")
    (k, k_b), (q, q_b), (v, v_b) = K, Q, V
    hs = slice(hk * 64, (hk + 1) * 64)
    nk = len(klist)
    NB = len(R.E)

    def finalize():
        oi = R.ocnt % 2
        R.ocnt += 1
        oTs, oTs_b, rr, rr_b = R.oTs2[oi], R.oTs2_b[oi], R.rr2[oi], R.rr2_b[oi]
        kb.op("act", lambda e: e.activation(out=oTs[0:65, :], in_=R.psO[hk][0:65, :], func=AF.Copy), reads=[R.psO_b[hk]], writes=[oTs_b])
        if extra_den is not None:
            ed_ap, ed_b = extra_den
            kb.op("dve", lambda e: e.tensor_tensor(out=oTs[64:65, :], in0=oTs[64:65, :], in1=ed_ap, op=ALU.add),
                  reads=[oTs_b, ed_b], writes=[oTs_b])
        if tiny is not None:
            kb.op("dve", lambda e: e.tensor_scalar(out=oTs[64:65, :], in0=oTs[64:65, :], scalar1=tiny, scalar2=None, op0=ALU.max),
                  reads=[oTs_b], writes=[oTs_b])
        kb.op("act", lambda e: e.activation(out=rr[64:65, :], in_=oTs[64:65, :], func=AF.Ln), reads=[oTs_b], writes=[rr_b])
        kb.op("act", lambda e: e.activation(out=rr[64:65, :], in_=rr[64:65, :], func=AF.Exp, scale=-1.0), reads=[rr_b], writes=[rr_b])
        kb.op("pe", lambda e: e.matmul(R.psD[0:64, :], lhsT=R.ones32[64:65, 0:64], rhs=rr[64:65, :], start=True, stop=True),
              reads=[R.ones32_b, rr_b], writes=[R.psD_b])
        kb.op("dve", lambda e: e.tensor_tensor(out=R.onb[oi][:], in0=oTs[0:64, :], in1=R.psD[0:64, :], op=ALU.mult),
              reads=[oTs_b, R.psD_b], writes=[R.onb_b[oi]])
        post(R.onb[oi], R.onb_b[oi])

    for idx, j in enumerate(klist):
        def front(idx=idx, j=j):
            pi = R.pcnt % 2
            R.pcnt += 1
            si = R.cnt % NB
            R.cnt += 1
            ksl = slice(j * 128, (j + 1) * 128)
            ms = maskfn(j) or []
            psv = R.psS[pi][:].rearrange("p (g q) -> p g q", g=4)
            if st_override is not None:
                l_ap0, r_ap0, bufs0 = st_override(j)
                kb.op("pe", lambda e: e.matmul(psv, lhsT=l_ap0, rhs=r_ap0, start=True, stop=(len(ms) == 0)),
                      reads=list(bufs0), writes=[R.psS_b[pi]])
            else:
                kb.op("pe", lambda e: e.matmul(psv, lhsT=k[hs, ksl], rhs=q[hs, :, qsl], start=True, stop=(len(ms) == 0)),
                      reads=[k_b, q_b], writes=[R.psS_b[pi]])
            for mi, (l_ap, r_ap, bufs) in enumerate(ms):
                kb.op("pe", lambda e: e.matmul(psv, lhsT=l_ap, rhs=r_ap.unsqueeze(1).to_broadcast([r_ap.shape[0], 4, 128]),
                                               start=False, stop=(mi == len(ms) - 1)),
                      reads=list(bufs), writes=[R.psS_b[pi]])
            kb.op("act", lambda e: e.activation(out=R.E[si][:], in_=R.psS[pi][:], func=AF.Exp, scale=0.125),
                  reads=[R.psS_b[pi]], writes=[R.E_b[si]])
            return si

        def back(idx=idx, j=j, si_box=None):
            pass

        box = {}

        def front2(front=front, box=box):
            box["si"] = front()

        def back2(idx=idx, j=j, box=box):
            si = box["si"]
            kb.op("pe", lambda e: e.matmul(R.psO[hk][0:65, :], lhsT=v[:, j, hk, :], rhs=R.E[si][:], start=(idx == 0), stop=(idx == nk - 1)),
                  reads=[v_b, R.E_b[si]], writes=[R.psO_b[hk]])
            if idx == nk - 1:
                finalize()

        R.pipe.push(front2, back2)


NIT = 20
MASKB = 30000.0
BIS_DVE_FRAC = 0.5
IDX_ENG = "dve"


def phase_dsa(nc, kb, l, cin, QTd, KTd, IqTd, IkTd, Vtok, gciw, OTd, qtiles=None):
    with ExitStack() as es:
        S = _Scope(nc, es)
        Q, K, V = _load_qkv(nc, kb, S, "a", QTd, KTd, Vtok, 0)
        iq = S.sb("iq", [128, 2, T], BF16); iq_b = kb.buf(dma=True)
        for c in range(2):
            kb.dma("sp", iq[:, c, :], IqTd[:, c, :], writes=[iq_b])
        ik = S.sb("ik", [128, T], BF16); ik_b = kb.buf(dma=True)
        kb.dma("sp", ik[:], IkTd, writes=[ik_b])
        iw = S.sb("iw", [128, NT, 4], F32); iw_b = kb.buf(dma=True)
        kb.dma("sp", iw[:], gciw[:, 24:28].rearrange("(kt p) d -> p kt d", p=128), writes=[iw_b])
        ident = S.sb("ident", [128, 128], BF16); ident_b = kb.buf(dma=True)
        kb.dma("sp", ident[:], cin["c_ident"], writes=[ident_b])
        negd = S.sb("negd", [128, 128], F32); negd_b = kb.buf(dma=True)
        kb.dma("sp", negd[:], cin["c_negdiag"], writes=[negd_b])
        p2 = S.sb("p2", [128, 24], F32); p2_b = kb.buf(dma=True)
        kb.dma("sp", p2[:], cin["c_p2"], writes=[p2_b])
        R = AttnRes(nc, kb, S)
        psI = [S.ps("psI%d" % i) for i in range(2)]
        psI_b = kb.bufs(2, excl=True)
        psT = S.ps("psT", [128, 8, 128], BF16)
        psT_b = kb.buf(excl=True)
        score = S.sb("score", [128, T], F32); score_b = kb.buf()
        rl = [S.sb("rl%d" % i, [128, 512], F32) for i in range(2)]
        rl_b = kb.bufs(2)
        junk = S.sb("junkc", [128, T], BF16); junk_b = kb.buf()
        maskb = S.sb("maskb", [128, T], BF16); maskb_b = kb.buf()
        maskT = S.sb("maskT", [128, NT, 128], BF16); maskT_b = kb.buf()
        stt = S.sb("stt", [128, 8], F32); stt_b = kb.buf()
        sta = S.sb("sta", [128, 2], F32); sta_b = kb.buf()
        midt = S.sb("midt", [128, 2], F32); mid_b = kb.buf()
        junka = S.sb("junka", [128, T], BF16); junka_b = kb.buf()
        wt = S.sb("wt", [128, 24], F32); wt_b = kb.buf()
        icnt = [0]
        maskT2 = [maskT, S.sb("maskTb", [128, NT, 128], BF16)]
        maskT2_b = [maskT_b, kb.buf()]

        def mask_units(i, mb):
            n = (i + 1) * 128
            qsl = slice(i * 128, n)
            units = []

            def idx_unit(k0, w, h):
                def u():
                    pi = icnt[0] % 2
                    icnt[0] += 1
                    hh = slice((h % 2) * 64, (h % 2) * 64 + 64)
                    kb.op("pe", lambda e: e.matmul(psI[pi][:, 0:w], lhsT=iq[hh, h // 2, qsl], rhs=ik[hh, k0:k0 + w], start=True, stop=True),
                          reads=[iq_b, ik_b], writes=[psI_b[pi]])
                    kb.op("act", lambda e: e.activation(out=rl[pi][:, 0:w], in_=psI[pi][:, 0:w], func=AF.Relu, scale=0.0625),
                          reads=[psI_b[pi]], writes=[rl_b[pi]])
                    if h == 0:
                        kb.op(IDX_ENG, lambda e: e.tensor_scalar(out=score[:, k0:k0 + w], in0=rl[pi][:, 0:w], scalar1=iw[:, i, 0:1], scalar2=None, op0=ALU.mult),
                              reads=[rl_b[pi], iw_b], writes=[score_b])
                    else:
                        kb.op("dve", lambda e: e.scalar_tensor_tensor(out=score[:, k0:k0 + w], in0=rl[pi][:, 0:w], scalar=iw[:, i, h:h + 1],
                                                                      in1=score[:, k0:k0 + w], op0=ALU.mult, op1=ALU.add),
                              reads=[rl_b[pi], iw_b, score_b], writes=[score_b])
                return u

            for k0 in range(0, n, 512):
                w = min(512, n - k0)
                for h in range(4):
                    units.append(idx_unit(k0, w, h))

            def init_unit():
                kb.op("dve", lambda e: e.tensor_tensor(out=score[:, i * 128:n], in0=score[:, i * 128:n], in1=negd[:], op=ALU.add),
                      reads=[score_b, negd_b], writes=[score_b])
                if i < 2:
                    kb.op("dve", lambda e: e.memset(midt[:, 0:1], -1e29), writes=[mid_b])
                else:
                    kb.op("dve", lambda e: e.tensor_reduce(out=stt[:, 0:1], in_=score[:, 0:n], axis=AX.X, op=ALU.max), reads=[score_b], writes=[stt_b])
                    kb.op("dve", lambda e: e.tensor_reduce(out=stt[:, 1:2], in_=score[:, 0:i * 128], axis=AX.X, op=ALU.min), reads=[score_b], writes=[stt_b])
                    kb.op("dve", lambda e: e.tensor_scalar(out=stt[:, 1:2], in0=stt[:, 1:2], scalar1=-1.0, scalar2=None, op0=ALU.add), reads=[stt_b], writes=[stt_b])
                    kb.op("dve", lambda e: e.tensor_tensor(out=stt[:, 2:3], in0=stt[:, 0:1], in1=stt[:, 1:2], op=ALU.subtract), reads=[stt_b], writes=[stt_b])
                    kb.op("dve", lambda e: e.tensor_scalar(out=wt[:], in0=p2[:], scalar1=stt[:, 2:3], scalar2=None, op0=ALU.mult), reads=[stt_b, p2_b], writes=[wt_b])
                    kb.op("dve", lambda e: e.tensor_tensor(out=midt[:, 0:1], in0=stt[:, 1:2], in1=wt[:, 0:1], op=ALU.add), reads=[stt_b, wt_b], writes=[mid_b])
            units.append(init_unit)

            def bis_unit(it):
                n_d = int((n * BIS_DVE_FRAC) // 128) * 128 if DBG.get("bis_split", True) else n
                m_a = n - n_d

                def u():
                    if m_a > 0:
                        kb.op("act", lambda e: e.activation(out=junka[:, 0:m_a], in_=score[:, n_d:n], func=AF.Sign, scale=-1.0, bias=midt[:, 0:1],
                                                            accum_out=sta[:, 0:1]),
                              reads=[score_b, mid_b], writes=[junka_b, sta_b])
                    kb.op("dve", lambda e: e.tensor_scalar(out=junk[:, 0:n_d], in0=score[:, 0:n_d], scalar1=midt[:, 0:1], scalar2=0.0, op0=ALU.is_gt, op1=ALU.add,
                                                           accum_out=stt[:, 4:5]),
                          reads=[score_b, mid_b], writes=[junk_b, stt_b])
                    if m_a > 0:
                        kb.op("dve", lambda e: e.scalar_tensor_tensor(out=stt[:, 4:5], in0=sta[:, 0:1], scalar=-0.5, in1=stt[:, 4:5], op0=ALU.mult, op1=ALU.add),
                              reads=[sta_b, stt_b], writes=[stt_b])
                    kb.op("dve", lambda e: e.tensor_scalar(out=stt[:, 5:6], in0=stt[:, 4:5], scalar1=255.5 - 0.5 * m_a, scalar2=wt[:, it:it + 1], op0=ALU.is_gt, op1=ALU.mult),
                          reads=[stt_b, wt_b], writes=[stt_b])
                    kb.op("dve", lambda e: e.scalar_tensor_tensor(out=midt[:, 0:1], in0=midt[:, 0:1], scalar=wt[:, it + 1:it + 2], in1=stt[:, 5:6],
                                                                  op0=ALU.subtract, op1=ALU.add),
                          reads=[stt_b, wt_b, mid_b], writes=[mid_b])
                return u
            if i >= 2:
                for it in range(NIT):
                    units.append(bis_unit(it))

            def fin_unit():
                if i >= 2:
                    kb.op("dve", lambda e: e.tensor_tensor(out=midt[:, 0:1], in0=midt[:, 0:1], in1=wt[:, NIT:NIT + 1], op=ALU.subtract), reads=[mid_b, wt_b], writes=[mid_b])
                kb.op("dve", lambda e: e.tensor_scalar(out=maskb[:, 0:n], in0=score[:, 0:n], scalar1=midt[:, 0:1], scalar2=None, op0=ALU.is_gt),
                      reads=[score_b, mid_b], writes=[maskb_b])
            units.append(fin_unit)

            def tr_unit(j0, nj):
                def u():
                    for jj in range(nj):
                        j = j0 + jj
                        kb.op("pe", lambda e: e.transpose(out=psT[:, jj, :], in_=maskb[:, j * 128:(j + 1) * 128], identity=ident[:]),
                              reads=[maskb_b, ident_b], writes=[psT_b])
                    kb.op("act", lambda e: e.activation(out=maskT2[mb][:, j0:j0 + nj, :], in_=psT[:, 0:nj, :], func=AF.Identity, scale=MASKB, bias=-MASKB),
                          reads=[psT_b], writes=[maskT2_b[mb]])
                return u
            for j0 in range(0, i + 1, 8):
                units.append(tr_unit(j0, min(8, i + 1 - j0)))
            return units

        tiles = list(range(NT) if qtiles is None else qtiles)
        for u in mask_units(tiles[0], 0):
            u()
        for ti, i in enumerate(tiles):
            n = (i + 1) * 128
            qsl = slice(i * 128, n)
            mb = ti % 2
            nxt = mask_units(tiles[ti + 1], (ti + 1) % 2) if ti + 1 < len(tiles) else []
            n_idx = 4 * len(range(0, (tiles[ti + 1] + 1) * 128, 512)) if ti + 1 < len(tiles) else 0
            R.pipe.set_bg(nxt, 2 * (i + 1), burst=4, nburst=n_idx)
            for hk in range(2):
                attn_tile(kb, R, hk, qsl, list(range(i + 1)), K, Q, V, lambda j, mb=mb: [(ident[:], maskT2[mb][:, j, :], [ident_b, maskT2_b[mb]])],
                          post=lambda o, o_b, hk=hk, qsl=qsl: kb.dma("sp", OTd[0:64, hk * 4:(hk + 1) * 4, qsl],
                                                                      o[:].rearrange("p (g q) -> p g q", g=4), reads=[o_b]))
            R.pipe.flush()
            R.pipe.flush_bg()
        kb.barrier()


def phase_swa(nc, kb, l, cin, sinks, QTd, KTd, Vtok, OTd, qtiles=None):
    with ExitStack() as es:
        S = _Scope(nc, es)
        Q, K, V = _load_qkv(nc, kb, S, "b", QTd, KTd, Vtok, 1)
        R = AttnRes(nc, kb, S)
        md = S.sb("md", [128, 128], BF16); md_b = kb.buf(dma=True)
        kb.dma("sp", md[:], cin["c_mdiagb"], writes=[md_b])
        mp = S.sb("mp", [128, 128], BF16); mp_b = kb.buf(dma=True)
        kb.dma("sp", mp[:], cin["c_mprevb"], writes=[mp_b])
        ident = S.sb("ident", [128, 128], BF16); ident_b = kb.buf(dma=True)
        kb.dma("sp", ident[:], cin["c_ident"], writes=[ident_b])
        sk = S.sb("sk", [65, 8], F32); sk_b = kb.buf(dma=True)
        kb.dma("sp", sk[64:65, :], sinks[l:l + 1, :], writes=[sk_b])
        kb.op("act", lambda e: e.activation(out=sk[64:65, :], in_=sk[64:65, :], func=AF.Exp), reads=[sk_b], writes=[sk_b])
        esb = S.sb("esb", [65, 8, 128], F32); esb_b = kb.buf()
        kb.op("dve", lambda e: e.tensor_copy(out=esb[64:65, :, :], in_=sk[64:65, :].unsqueeze(2).to_broadcast([1, 8, 128])), reads=[sk_b], writes=[esb_b])
        for i in (range(NT) if qtiles is None else qtiles):
            qsl = slice(i * 128, (i + 1) * 128)
            klist = [j for j in (i - 1, i) if j >= 0]
            for hk in range(2):
                attn_tile(kb, R, hk, qsl, klist, K, Q, V,
                          lambda j, i=i: [(ident[:], md[:], [ident_b, md_b])] if j == i else [(ident[:], mp[:], [ident_b, mp_b])],
                          extra_den=(esb[64:65, hk * 4:(hk + 1) * 4, :].rearrange("p g q -> p (g q)"), esb_b),
                          post=lambda o, o_b, hk=hk, qsl=qsl: kb.dma("sp", OTd[0:64, hk * 4:(hk + 1) * 4, qsl],
                                                                      o[:].rearrange("p (g q) -> p g q", g=4), reads=[o_b]))
        R.pipe.flush()
        kb.barrier()


def phase_cmp(nc, kb, l, KcTd, VcTd, peT, w_ck1, w_ck2, w_cv1, w_cv2, KcmpT, Vcmp):
    with ExitStack() as es:
        S = _Scope(nc, es)
        xc = [S.sb("xc%d" % i, [128, T], BF16) for i in range(2)]
        xc_b = kb.bufs(2, dma=True)
        kb.dma("sp", xc[0][:], KcTd, writes=[xc_b[0]])
        kb.dma("sp", xc[1][:], VcTd, writes=[xc_b[1]])
        w1 = [S.sb("w1_%d" % i, [128, 32, 256], BF16) for i in range(2)]
        w1_b = kb.bufs(2, dma="sw")
        pe = [S.sb("pe%d" % i, [128, 32], BF16) for i in range(2)]
        pe_b = kb.bufs(2, dma="sw")
        for si, wsrc in enumerate([w_ck1, w_cv1]):
            for half in range(2):
                kb.dma("pool", w1[si][half * 64:(half + 1) * 64, :, :], wsrc[l].rearrange("(l d) j -> d l j", d=64), writes=[w1_b[si]])
            kb.dma("pool", pe[si][:], peT[l, si], writes=[pe_b[si]])
        w2k = S.sb("w2k", [128, 2, 128], BF16); w2k_b = kb.buf(dma="sw")
        for half in range(2):
            kb.dma("pool", w2k[:, :, half * 64:(half + 1) * 64], w_ck2[l].rearrange("(jc j) d -> j jc d", j=128), writes=[w2k_b])
        w2v = S.sb("w2v", [128, 2, 64], BF16); w2v_b = kb.buf(dma="sw")
        kb.dma("pool", w2v[:], w_cv2[l].rearrange("(jc j) d -> j jc d", j=128), writes=[w2v_b])
        psB = S.ps("psB"); psB_b = kb.buf(excl=True)
        psH = [S.ps("psH%d" % i) for i in range(2)]; psH_b = kb.bufs(2, excl=True)
        psK = S.ps("psK"); psK_b = kb.buf(excl=True)
        b1s = S.sb("b1s", [128, 2], F32); b1s_b = kb.buf()
        hidb = S.sb("hidb", [128, 2, 256], BF16); hidb_b = kb.buf()
        kb.op("dve", lambda e: e.memset(hidb[:], 0.0), writes=[hidb_b])
        kcmp = S.sb("kcmp", [128, 256], BF16); kcmp_b = kb.buf(dma=True)
        kb.op("dve", lambda e: e.memset(kcmp[:], 0.0), writes=[kcmp_b])
        vcmp = S.sb("vcmp", [128, 2, 2, 64], BF16); vcmp_b = kb.buf(dma=True)
        kb.op("dve", lambda e: e.memset(vcmp[:], 0.0), writes=[vcmp_b])
        for si in range(2):
            for jc in range(2):
                for ll in range(32):
                    kb.op("pe", lambda e: e.matmul(psB[:, jc:jc + 1], lhsT=w1[si][0:64, ll, jc * 128:(jc + 1) * 128], rhs=pe[si][0:64, ll:ll + 1],
                                                   start=(ll == 0), stop=(ll == 31)),
                          reads=[w1_b[si], pe_b[si]], writes=[psB_b])
            kb.op("dve", lambda e: e.tensor_copy(out=b1s[:], in_=psB[:, 0:2]), reads=[psB_b], writes=[b1s_b])
            for hk in range(2):
                hs = slice(hk * 64, (hk + 1) * 64)
                for jc in range(2):
                    for ll in range(32):
                        kb.op("pe", lambda e: e.matmul(psH[jc][:, 0:255], lhsT=w1[si][hs, ll, jc * 128:(jc + 1) * 128],
                                                       rhs=xc[si][hs, ll:ll + 16 * 254 + 1:16], start=(ll == 0), stop=(ll == 31)),
                              reads=[w1_b[si], xc_b[si]], writes=[psH_b[jc]])
                    kb.op("act", lambda e: e.activation(out=hidb[:, jc, 0:255], in_=psH[jc][:, 0:255], func=AF.Relu, bias=b1s[:, jc:jc + 1], scale=1.0),
                          reads=[psH_b[jc], b1s_b], writes=[hidb_b])
                if si == 0:
                    for jc in range(2):
                        kb.op("pe", lambda e: e.matmul(psK[:, 0:255], lhsT=w2k[:, jc, :], rhs=hidb[:, jc, 0:255], start=(jc == 0), stop=(jc == 1)),
                              reads=[w2k_b, hidb_b], writes=[psK_b])
                    kb.op("act", lambda e: e.activation(out=kcmp[hs, 0:255], in_=psK[hs, 0:255], func=AF.Copy), reads=[psK_b], writes=[kcmp_b])
                else:
                    for ct in range(2):
                        nct = 128 if ct == 0 else 127
                        for jc in range(2):
                            kb.op("pe", lambda e: e.matmul(psK[0:nct, 0:64], lhsT=hidb[:, jc, ct * 128:ct * 128 + nct], rhs=w2v[:, jc, :],
                                                           start=(jc == 0), stop=(jc == 1)),
                                  reads=[w2v_b, hidb_b], writes=[psK_b])
                        kb.op("act", lambda e: e.activation(out=vcmp[0:nct, ct, hk, :], in_=psK[0:nct, 0:64], func=AF.Copy), reads=[psK_b], writes=[vcmp_b])
        kb.dma("sp", KcmpT, kcmp[:], reads=[kcmp_b])
        kb.dma("sp", Vcmp, vcmp[:], reads=[vcmp_b])
        kb.barrier()


def phase_nsa(nc, kb, l, cin, QTd, KsTd, KwTd, Vtok, KcmpT, Vcmp, sgcT, OTd, qtiles=None):
    with ExitStack() as es:
        S = _Scope(nc, es)
        Q, Ks, Vs = _load_qkv(nc, kb, S, "cs", QTd, KsTd, Vtok, 2)
        kw = S.sb("k_w", [128, T], BF16); kw_b = kb.buf(dma=True)
        kb.dma("sp", kw[:], KwTd, writes=[kw_b])
        Kw = (kw, kw_b)
        vw = S.sb("v_w", [128, NT, 2, 65], BF16); vw_b = kb.buf(dma=True)
        kb.op("pool", lambda e: e.memset(vw[:], 1.0), writes=[vw_b])
        for hk in range(2):
            kb.dma("sp", vw[:, :, hk, 0:64], Vtok[:, 3 * 128 + hk * 64: 3 * 128 + hk * 64 + 64].rearrange("(kt p) d -> p kt d", p=128), writes=[vw_b])
        Vw = (vw, vw_b)
        kcmp = S.sb("kcmp", [128, 256], BF16); kcmp_b = kb.buf(dma=True)
        kb.dma("sp", kcmp[:], KcmpT, writes=[kcmp_b])
        vcx = S.sb("vcx", [128, 2, 2, 65], BF16); vcx_b = kb.buf(dma=True)
        kb.op("pool", lambda e: e.memset(vcx[:], 1.0), writes=[vcx_b])
        kb.dma("sp", vcx[:, :, :, 0:64], Vcmp, writes=[vcx_b])
        ovx = S.sb("ovx", [128, 2, 65], BF16); ovx_b = kb.buf(dma=True)
        kb.op("pool", lambda e: e.memset(ovx[:], 1.0), writes=[ovx_b])
        kb.dma("sp", ovx[:, :, 0:64], cin["c_ovl"], writes=[ovx_b])
        mcmp = S.sb("mcmp", [128, 2, T], BF16); mcmp_b = kb.buf(dma=True)
        for ct in range(2):
            kb.dma("sp", mcmp[:, ct, :], cin["c_mcmp"][:, ct, :], writes=[mcmp_b])
        kbs = [S.sb("kbs%d" % i, [128, T], BF16) for i in range(2)]; kbs_b = kb.bufs(2, dma=True)
        for hk_ in range(2):
            hs_ = slice(hk_ * 64, (hk_ + 1) * 64)
            oh_ = slice((1 - hk_) * 64, (2 - hk_) * 64)
            kb.dma("sp", kbs[hk_][hs_, :], KsTd[hs_, :], writes=[kbs_b[hk_]])
            kb.dma("sp", kbs[hk_][oh_, :], cin["c_bsel"], writes=[kbs_b[hk_]])
        QB = [S.sb("QB%d" % i, [128, 4, 128], BF16) for i in range(2)]; QB_b = kb.bufs(2)
        f1 = S.sb("f1", [128, NT, 64], F32); f1_b = kb.buf(dma=True)
        kb.dma("sp", f1[:], cin["c_f1"], writes=[f1_b])
        f2 = S.sb("f2", [128, NT, 64], F32); f2_b = kb.buf(dma=True)
        kb.dma("sp", f2[:], cin["c_f2"], writes=[f2_b])
        sgc = S.sb("sgc", [32, T], F32); sgc_b = kb.buf(dma=True)
        kb.dma("sp", sgc[:], sgcT, writes=[sgc_b])
        gsel = S.sb("gsel", [32, 24, 64], F32); gsel_b = kb.buf(dma=True)
        kb.dma("sp", gsel[:], cin["c_gsel"], writes=[gsel_b])
        md = S.sb("md", [128, 128], BF16); md_b = kb.buf(dma=True)
        kb.dma("sp", md[:], cin["c_mdiagb"], writes=[md_b])
        mp = S.sb("mp", [128, 128], BF16); mp_b = kb.buf(dma=True)
        kb.dma("sp", mp[:], cin["c_mprevb"], writes=[mp_b])
        ident = S.sb("ident", [128, 128], BF16); ident_b = kb.buf(dma=True)
        kb.dma("sp", ident[:], cin["c_ident"], writes=[ident_b])
        R = AttnRes(nc, kb, S, out_dt=F32)
        psP = S.ps("psP"); psP_b = kb.buf(excl=True)
        psX = S.ps("psX"); psX_b = kb.buf(excl=True)
        psXT = S.ps("psXT", [128, 8, 128], BF16); psXT_b = kb.buf(excl=True)
        Emc = [S.sb("Emc%d" % i, [128, 512], BF16) for i in range(2)]; Emc_b = kb.bufs(2)
        Ec = [S.sb("Ec%d" % i, [128, 512], BF16) for i in range(2)]; Ec_b = kb.bufs(2)
        pp = S.sb("pp", [128, 4, 65], F32); pp_b = kb.buf()
        rec = S.sb("rec", [128, 4], F32); rec_b = kb.buf()
        imp = [S.sb("imp%d" % i, [128, 64], F32) for i in range(2)]; imp_b = kb.bufs(2)
        m8 = S.sb("m8", [128, 16], F32); m8_b = kb.buf()
        imp3 = S.sb("imp3", [128, 64], F32); imp3_b = kb.buf()
        bm2 = [S.sb("bm%d" % i, [128, 128], BF16) for i in range(2)]; bm2_b = kb.bufs(2)
        for i_ in range(2):
            kb.op("pool", lambda e: e.memset(bm2[i_][:], 0.0), writes=[bm2_b[i_]])

        acc = [S.sb("acc%d" % i, [64, 512], F32) for i in range(2)]; acc_b = kb.bufs(2)
        tmpo = S.sb("tmpo", [64, 512], F32); tmpo_b = kb.buf()
        tmpc = S.sb("tmpc", [64, 512], F32); tmpc_b = kb.buf()
        oTc = S.sb("oTc", [65, 512], F32); oTc_b = kb.buf()
        rrc = S.sb("rrc", [65, 512], F32); rrc_b = kb.buf()
        ocb = [S.sb("ocb%d" % i, [64, 512], BF16) for i in range(2)]; ocb_b = kb.bufs(2, dma=True)

        def gate_acc(hk, br, o, o_b, first, qsl):
            for g in range(4):
                r = (hk * 4 + g) * 3 + br
                kb.op("pe", lambda e: e.matmul(psX[0:64, g * 128:(g + 1) * 128], lhsT=gsel[:, r, :], rhs=sgc[:, qsl], start=True, stop=True),
                      reads=[gsel_b, sgc_b], writes=[psX_b])
            if first:
                kb.op("dve", lambda e: e.tensor_tensor(out=acc[hk][:], in0=o[:], in1=psX[0:64, :], op=ALU.mult), reads=[o_b, psX_b], writes=[acc_b[hk]])
            else:
                kb.op("dve", lambda e: e.tensor_tensor(out=tmpo[:], in0=o[:], in1=psX[0:64, :], op=ALU.mult), reads=[o_b, psX_b], writes=[tmpo_b])
                kb.op("pool", lambda e: e.tensor_tensor(out=acc[hk][:], in0=acc[hk][:], in1=tmpo[:], op=ALU.add), reads=[tmpo_b, acc_b[hk]], writes=[acc_b[hk]])

        def sel_units(i, hk):
            qsl = slice(i * 128, (i + 1) * 128)
            nct = 1 if 8 * i + 6 < 128 else 2
            hs = slice(hk * 64, (hk + 1) * 64)
            oh = slice((1 - hk) * 64, (2 - hk) * 64)
            units = []

            def u_scores(ct):
                def u():
                    pi = R.pcnt % 2
                    R.pcnt += 1
                    kb.op("pe", lambda e: e.matmul(R.psS[pi][:].rearrange("p (g q) -> p g q", g=4), lhsT=kcmp[hs, ct * 128:(ct + 1) * 128],
                                                   rhs=Q[0][hs, :, qsl], start=True, stop=True),
                          reads=[kcmp_b, Q[1]], writes=[R.psS_b[pi]])
                    kb.op("act", lambda e: e.activation(out=Ec[ct][:], in_=R.psS[pi][:], func=AF.Exp, scale=0.125), reads=[R.psS_b[pi]], writes=[Ec_b[ct]])
                    kb.op("pool", lambda e: e.tensor_tensor(out=Emc[ct][:].rearrange("p (g q) -> p g q", g=4),
                                                            in0=Ec[ct][:].rearrange("p (g q) -> p g q", g=4),
                                                            in1=mcmp[:, ct, qsl].unsqueeze(1).to_broadcast([128, 4, 128]), op=ALU.mult),
                          reads=[Ec_b[ct], mcmp_b], writes=[Emc_b[ct]])
                return u
            for ct in range(nct):
                units.append(u_scores(ct))

            def u_pv():
                for ct in range(nct):
                    kb.op("pe", lambda e: e.matmul(psP[0:65, :], lhsT=vcx[:, ct, hk, :], rhs=Emc[ct][:], start=(ct == 0), stop=(ct == nct - 1)),
                          reads=[vcx_b, Emc_b[ct]], writes=[psP_b])
                kb.op("act", lambda e: e.activation(out=oTc[0:65, :], in_=psP[0:65, :], func=AF.Copy), reads=[psP_b], writes=[oTc_b])
                for g in range(4):
                    for ct in range(nct):
                        kb.op("pe", lambda e: e.matmul(psP[:, g * 65:(g + 1) * 65], lhsT=Emc[ct][:, g * 128:(g + 1) * 128], rhs=ovx[:, ct, :],
                                                       start=(ct == 0), stop=(ct == nct - 1)),
                              reads=[Emc_b[ct], ovx_b], writes=[psP_b])
                kb.op("act", lambda e: e.activation(out=pp[:].rearrange("p g c -> p (g c)"), in_=psP[:, 0:260], func=AF.Copy), reads=[psP_b], writes=[pp_b])
            units.append(u_pv)

            def u_ocmp():
                kb.op("dve", lambda e: e.tensor_scalar(out=oTc[64:65, :], in0=oTc[64:65, :], scalar1=1e-30, scalar2=None, op0=ALU.max), reads=[oTc_b], writes=[oTc_b])
                kb.op("act", lambda e: e.activation(out=rrc[64:65, :], in_=oTc[64:65, :], func=AF.Ln), reads=[oTc_b], writes=[rrc_b])
                kb.op("act", lambda e: e.activation(out=rrc[64:65, :], in_=rrc[64:65, :], func=AF.Exp, scale=-1.0), reads=[rrc_b], writes=[rrc_b])
                kb.op("pe", lambda e: e.matmul(psX[0:64, :], lhsT=R.ones32[64:65, 0:64], rhs=rrc[64:65, :], start=True, stop=True),
                      reads=[R.ones32_b, rrc_b], writes=[psX_b])
                kb.op("dve", lambda e: e.tensor_tensor(out=tmpc[:], in0=oTc[0:64, :], in1=psX[0:64, :], op=ALU.mult), reads=[oTc_b, psX_b], writes=[tmpc_b])
                gate_acc(hk, 0, tmpc, tmpc_b, True, qsl)
            units.append(u_ocmp)

            def u_imp():
                kb.op("dve", lambda e: e.tensor_scalar(out=rec[:], in0=pp[:, :, 64], scalar1=1e-30, scalar2=None, op0=ALU.max), reads=[pp_b], writes=[rec_b])
                kb.op("dve", lambda e: e.reciprocal(out=rec[:], in_=rec[:]), reads=[rec_b], writes=[rec_b])
                kb.op("dve", lambda e: e.tensor_scalar(out=imp[hk][:], in0=pp[:, 0, 0:64], scalar1=rec[:, 0:1], scalar2=None, op0=ALU.mult), reads=[pp_b, rec_b], writes=[imp_b[hk]])
                for g in range(1, 4):
                    kb.op("dve", lambda e: e.scalar_tensor_tensor(out=imp[hk][:], in0=pp[:, g, 0:64], scalar=rec[:, g:g + 1], in1=imp[hk][:], op0=ALU.mult, op1=ALU.add),
                          reads=[pp_b, rec_b, imp_b[hk]], writes=[imp_b[hk]])
                kb.op("dve", lambda e: e.tensor_tensor(out=imp[hk][:], in0=imp[hk][:], in1=f1[:, i, :], op=ALU.max), reads=[imp_b[hk], f1_b], writes=[imp_b[hk]])
                kb.op("dve", lambda e: e.tensor_tensor(out=imp[hk][:], in0=imp[hk][:], in1=f2[:, i, :], op=ALU.min), reads=[imp_b[hk], f2_b], writes=[imp_b[hk]])
            units.append(u_imp)

            def u_top():
                kb.op("dve", lambda e: e.max(out=m8[:, 0:8], in_=imp[hk][:]), reads=[imp_b[hk]], writes=[m8_b])
                kb.op("dve", lambda e: e.match_replace(out=imp3[:], in_to_replace=m8[:, 0:8], in_values=imp[hk][:], imm_value=-3e38), reads=[imp_b[hk], m8_b], writes=[imp3_b])
                kb.op("dve", lambda e: e.max(out=m8[:, 8:16], in_=imp3[:]), reads=[imp3_b], writes=[m8_b])
                kb.op("dve", lambda e: e.tensor_scalar(out=bm2[hk][:, oh], in0=imp[hk][:], scalar1=m8[:, 15:16], scalar2=None, op0=ALU.is_ge),
                      reads=[imp_b[hk], m8_b], writes=[bm2_b[hk]])
            units.append(u_top)

            def u_tr():
                kb.op("pe", lambda e: e.transpose(out=psXT[:, 0, :], in_=bm2[hk][:], identity=ident[:]), reads=[bm2_b[hk], ident_b], writes=[psXT_b])
                kb.op("act", lambda e: e.activation(out=QB[hk][oh, :, :], in_=psXT[oh, 0, :].unsqueeze(1).to_broadcast([64, 4, 128]),
                                                    func=AF.Identity, scale=MASKB, bias=-MASKB),
                      reads=[psXT_b], writes=[QB_b[hk]])
                kb.op("pool", lambda e: e.tensor_copy(out=QB[hk][hs, :, :], in_=Q[0][hs, :, qsl]), reads=[Q[1]], writes=[QB_b[hk]])
            units.append(u_tr)
            return units

        order = [(i, hk) for i in (range(NT) if qtiles is None else qtiles) for hk in range(2)]
        for u in sel_units(*order[0]):
            u()
        for oi, (i, hk) in enumerate(order):
            qsl = slice(i * 128, (i + 1) * 128)
            wl = list(range(max(0, i - 4), i + 1))
            nxt = sel_units(*order[oi + 1]) if oi + 1 < len(order) else []
            R.pipe.set_bg(nxt, (i + 1) + len(wl))

            def slc_mask(j, i=i, hk=hk):
                return [(ident[:], md[:], [ident_b, md_b])] if j == i else None

            def slc_st(j, hk=hk):
                return kbs[hk][:, j * 128:(j + 1) * 128], QB[hk][:], [kbs_b[hk], QB_b[hk]]

            attn_tile(kb, R, hk, qsl, list(range(i + 1)), Ks, Q, Vs, slc_mask, st_override=slc_st,
                      post=lambda o, o_b, hk=hk, qsl=qsl: gate_acc(hk, 1, o, o_b, False, qsl))

            def win_post(o, o_b, hk=hk, qsl=qsl):
                gate_acc(hk, 2, o, o_b, False, qsl)
                kb.op("act", lambda e: e.activation(out=ocb[hk][:], in_=acc[hk][:], func=AF.Copy), reads=[acc_b[hk]], writes=[ocb_b[hk]])
                kb.dma("sp", OTd[0:64, hk * 4:(hk + 1) * 4, qsl], ocb[hk][:].rearrange("p (g q) -> p g q", g=4), reads=[ocb_b[hk]])

            attn_tile(kb, R, hk, qsl, wl, Kw, Q, Vw,
                      lambda j, i=i: [(ident[:], md[:], [ident_b, md_b])] if j == i else ([(ident[:], mp[:], [ident_b, mp_b])] if j == i - 4 else None),
                      post=win_post)
            R.pipe.flush()
            R.pipe.flush_bg()
        kb.barrier()


def phase_merge(nc, kb, l, x_src, x_dst, OTd, sgT, w_branch, w_out):
    with ExitStack() as es:
        S = _Scope(nc, es)
        wb = S.sb("wb", [128, 3, 4, D], BF16); wb_b = kb.buf(dma="sw")
        for n in range(3):
            kb.dma("pool", wb[:, n, :, :], w_branch[l, n].rearrange("(c p) o -> p c o", p=128), writes=[wb_b])
        wo = S.sb("wo", [128, 8, D], BF16); wo_b = kb.buf(dma="sw")
        for kc in range(8):
            kb.dma("pool", wo[:, kc, :], w_out[l, kc * 128:(kc + 1) * 128, :], writes=[wo_b])
        sg = [S.sb("sg%d" % i, [128, 24, 512], BF16) for i in range(2)]; sg_b = kb.bufs(2, dma=True)
        ot = [S.sb("ot%d" % i, [128, 3, 4, 512], BF16) for i in range(2)]; ot_b = kb.bufs(2, dma=True)
        xt = [S.sb("xt%d" % i, [128, D], F32) for i in range(2)]; xt_b = kb.bufs(2, dma=True)
        mT = S.sb("mT", [128, 8, 512], BF16); mT_b = kb.buf()
        t0a = [S.sb("t0_%d" % i, [128, 512], F32) for i in range(3)]; t0a_b = kb.bufs(3)
        psY6 = [S.ps("psY%d" % i) for i in range(6)]; psY6_b = kb.bufs(6, excl=True)
        psXo = [S.ps("psXo%d" % i) for i in range(2)]; psXo_b = kb.bufs(2, excl=True)
        t06 = [S.sb("t0b_%d" % i, [128, 512], F32) for i in range(3)]; t06_b = kb.bufs(3)
        xcnt = 0
        for tg in range(8):
            gi = tg % 2
            tsl = slice(tg * 512, (tg + 1) * 512)
            for c in range(24):
                kb.dma("sp", sg[gi][:, c, :], sgT[:, c, tsl], writes=[sg_b[gi]])
            for n in range(3):
                for par in range(2):
                    kb.dma("sp", ot[gi][par * 64:(par + 1) * 64, n, :, :], OTd[n][0:64, par::2, tsl], writes=[ot_b[gi]])
            for oc in range(8):
                psY, psY_b = psY6[(oc % 2) * 3:(oc % 2) * 3 + 3], psY6_b[(oc % 2) * 3:(oc % 2) * 3 + 3]
                t0, t0_b = (t0a, t0a_b) if oc % 2 == 0 else (t06, t06_b)
                for n in range(3):
                    for c in range(4):
                        kb.op("pe", lambda e: e.matmul(psY[n][:], lhsT=wb[:, n, c, oc * 128:(oc + 1) * 128], rhs=ot[gi][:, n, c, :], start=(c == 0), stop=(c == 3)),
                              reads=[wb_b, ot_b[gi]], writes=[psY_b[n]])
                    kb.op("dve", lambda e: e.tensor_tensor(out=t0[n][:], in0=psY[n][:], in1=sg[gi][:, n * 8 + oc, :], op=ALU.mult),
                          reads=[psY_b[n], sg_b[gi]], writes=[t0_b[n]])
                kb.op("pool", lambda e: e.tensor_tensor(out=t0[0][:], in0=t0[0][:], in1=t0[1][:], op=ALU.add), reads=[t0_b[0], t0_b[1]], writes=[t0_b[0]])
                kb.op("pool", lambda e: e.tensor_tensor(out=mT[:, oc, :], in0=t0[0][:], in1=t0[2][:], op=ALU.add), reads=[t0_b[0], t0_b[2]], writes=[mT_b])
            for tt in range(4):
                xi = xcnt % 2
                xcnt += 1
                rows = slice(tg * 512 + tt * 128, tg * 512 + (tt + 1) * 128)
                kb.dma("sp", xt[xi][:], x_src[rows, :], writes=[xt_b[xi]])
                for half in range(2):
                    pb = half
                    for kc in range(8):
                        kb.op("pe", lambda e: e.matmul(psXo[pb][:], lhsT=mT[:, kc, tt * 128:(tt + 1) * 128], rhs=wo[:, kc, half * 512:(half + 1) * 512],
                                                       start=(kc == 0), stop=(kc == 7)),
                              reads=[mT_b, wo_b], writes=[psXo_b[pb]])
                    kb.op("dve", lambda e: e.tensor_tensor(out=xt[xi][:, half * 512:(half + 1) * 512], in0=xt[xi][:, half * 512:(half + 1) * 512],
                                                           in1=psXo[pb][:], op=ALU.add),
                          reads=[xt_b[xi], psXo_b[pb]], writes=[xt_b[xi]])
                kb.dma("sp", x_dst[rows, :], xt[xi][:], reads=[xt_b[xi]])
        kb.barrier()


def phase_mlp(nc, kb, l, x_src, x_dst, norm_mlp, w_up, w_down, cin):
    with ExitStack() as es:
        S = _Scope(nc, es)
        wu = S.sb("wu", [128, 8, D_FF], BF16); wu_b = kb.buf(dma="sw")
        for kc in range(8):
            kb.dma("pool", wu[:, kc, :], w_up[l, kc * 128:(kc + 1) * 128, :], writes=[wu_b])
        wd = S.sb("wd", [128, 32, D], BF16); wd_b = kb.buf(dma="sw")
        for c4 in range(8):
            kb.dma("pool", wd[:, c4 * 4:(c4 + 1) * 4, :], w_down[l, c4 * 512:(c4 + 1) * 512, :].rearrange("(c p) o -> p c o", p=128), writes=[wd_b])
        gt = S.sb("gt", [128, D], F32); gt_b = kb.buf(dma=True)
        kb.dma("sp", gt[:], norm_mlp[l].partition_broadcast(128), writes=[gt_b])
        ident = S.sb("ident", [128, 128], BF16); ident_b = kb.buf(dma=True)
        kb.dma("sp", ident[:], cin["c_ident"], writes=[ident_b])
        xt = [S.sb("xt%d" % i, [128, D], F32) for i in range(4)]; xt_b = kb.bufs(4, dma=True)
        junk = S.sb("junk", [128, D], BF16); junk_b = kb.buf()
        st = [S.sb("st%d" % i, [128, 4], F32) for i in range(2)]; st_b = kb.bufs(2)
        hb = [S.sb("hb%d" % i, [128, D], BF16) for i in range(2)]; hb_b = kb.bufs(2)
        h2T = S.sb("h2T", [128, 8, 256], BF16); h2T_b = kb.buf()
        uT = S.sb("uT", [128, 32, 256], BF16); uT_b = kb.buf()
        rr = [S.sb("rr%d" % i, [128, 256], F32) for i in range(2)]; rr_b = kb.bufs(2)
        pst = [S.ps("pst%d" % i, [128, 8, 128], BF16) for i in range(2)]; pst_b = kb.bufs(2, excl=True)
        psU = [S.ps("psU%d" % i) for i in range(2)]; psU_b = kb.bufs(2, excl=True)
        psDn = [S.ps("psDn%d" % i) for i in range(4)]; psDn_b = kb.bufs(4, excl=True)
        ucnt = 0
        for t2 in range(T // 256):
            for tt in range(2):
                xi = (t2 % 2) * 2 + tt
                i = tt
                rows = slice(t2 * 256 + tt * 128, t2 * 256 + (tt + 1) * 128)
                kb.dma("sp", xt[xi][:], x_src[rows, :], writes=[xt_b[xi]])
                kb.op("act", lambda e: e.activation(out=junk[:], in_=xt[xi][:], func=AF.Square, accum_out=st[i][:, 0:1]),
                      reads=[xt_b[xi]], writes=[junk_b, st_b[i]])
                kb.op("act", lambda e: e.activation(out=st[i][:, 1:2], in_=st[i][:, 0:1], func=AF.Sqrt, bias=EPS, scale=1.0 / D), reads=[st_b[i]], writes=[st_b[i]])
                kb.op("dve", lambda e: e.reciprocal(out=st[i][:, 2:3], in_=st[i][:, 1:2]), reads=[st_b[i]], writes=[st_b[i]])
                kb.op("dve", lambda e: e.scalar_tensor_tensor(out=hb[i][:], in0=xt[xi][:], scalar=st[i][:, 2:3], in1=gt[:], op0=ALU.mult, op1=ALU.mult),
                      reads=[xt_b[xi], st_b[i], gt_b], writes=[hb_b[i]])
                for kc in range(8):
                    kb.op("pe", lambda e: e.transpose(out=pst[i][:, kc, :], in_=hb[i][:, kc * 128:(kc + 1) * 128], identity=ident[:]),
                          reads=[hb_b[i], ident_b], writes=[pst_b[i]])
                kb.op("act", lambda e: e.activation(out=h2T[:, :, tt * 128:(tt + 1) * 128], in_=pst[i][:], func=AF.Copy), reads=[pst_b[i]], writes=[h2T_b])
            for ffc in range(32):
                ui = ucnt % 2
                ucnt += 1
                for kc in range(8):
                    kb.op("pe", lambda e: e.matmul(psU[ui][:, 0:256], lhsT=wu[:, kc, ffc * 128:(ffc + 1) * 128], rhs=h2T[:, kc, :], start=(kc == 0), stop=(kc == 7)),
                          reads=[wu_b, h2T_b], writes=[psU_b[ui]])
                kb.op("act", lambda e: e.activation(out=rr[ui][:], in_=psU[ui][:, 0:256], func=AF.Relu), reads=[psU_b[ui]], writes=[rr_b[ui]])
                kb.op("pool", lambda e: e.tensor_tensor(out=uT[:, ffc, :], in0=rr[ui][:], in1=rr[ui][:], op=ALU.mult), reads=[rr_b[ui]], writes=[uT_b])
            for tt in range(2):
                xi = (t2 % 2) * 2 + tt
                rows = slice(t2 * 256 + tt * 128, t2 * 256 + (tt + 1) * 128)
                for half in range(2):
                    pb = tt * 2 + half
                    for ffc in range(32):
                        kb.op("pe", lambda e: e.matmul(psDn[pb][:], lhsT=uT[:, ffc, tt * 128:(tt + 1) * 128], rhs=wd[:, ffc, half * 512:(half + 1) * 512],
                                                       start=(ffc == 0), stop=(ffc == 31)),
                              reads=[uT_b, wd_b], writes=[psDn_b[pb]])
                    kb.op("dve", lambda e: e.tensor_tensor(out=xt[xi][:, half * 512:(half + 1) * 512], in0=xt[xi][:, half * 512:(half + 1) * 512],
                                                           in1=psDn[pb][:], op=ALU.add),
                          reads=[xt_b[xi], psDn_b[pb]], writes=[xt_b[xi]])
                kb.dma("sp", x_dst[rows, :], xt[xi][:], reads=[xt_b[xi]])
        kb.barrier()
```

```python
import numpy as np
from contextlib import ExitStack
import concourse.bass as bass
import concourse.mybir as mybir
from concourse.bass_utils import run_bass_kernel_spmd
import ml_dtypes

F32 = mybir.dt.float32
BF16 = mybir.dt.bfloat16
ALU = mybir.AluOpType
AF = mybir.ActivationFunctionType
AX = mybir.AxisListType

T = 4096
D = 1024
NT = T // 128
DEPTH = 2
D_IN = 6236
D_FF = 4096
EPS = 1e-6
NEG = -1e30
N_CORES = 8

OFF = dict(qa=0, ka=512, va=640, iq=768, ik=1024, iw=1088, qb=1092, kb=1604, vb=1732,
           qc=1860, kc=2372, vc=2500, ks=2628, vs=2756, kw=2884, vw=3012, gc=3140, gm=3164)


DBG = {}
_UNIQ = [0]


def _uniq(name):
    _UNIQ[0] += 1
    return "%s_%d" % (name, _UNIQ[0])


class Buf:
    __slots__ = ("w", "r", "dsi", "excl")

    def __init__(self, excl=False):
        self.w = None
        self.r = {}
        self.dsi = None
        self.excl = excl


class Eng:
    def __init__(self, name, h, si, selfsync):
        self.name, self.h, self.si, self.selfsync = name, h, si, selfsync
        self.seen = {}


class KB:
    def __init__(self, nc, es, n_dma_sems=72):
        self.nc = nc
        self.sems = []
        self.semval = []
        self.engs = {}
        for name, h, ss in [("pe", nc.tensor, False), ("act", nc.scalar, True), ("dve", nc.vector, True),
                            ("pool", nc.gpsimd, True), ("sp", nc.sync, False)]:
            sem = es.enter_context(nc.semaphore("sem_" + name))
            self.sems.append(sem)
            self.semval.append(0)
            self.engs[name] = Eng(name, h, len(self.sems) - 1, ss)
        self.bar_si = len(self.sems)
        self.sems.append(es.enter_context(nc.semaphore("sem_bar")))
        self.semval.append(0)
        self.dma_free = []
        self.dma_free_sw = []
        for i in range(n_dma_sems):
            self.sems.append(es.enter_context(nc.semaphore("dsem%d" % i)))
            self.semval.append(0)
            (self.dma_free_sw if i < 8 else self.dma_free).append(len(self.sems) - 1)
        self.sw_sems = set(self.dma_free_sw)
        self.phase_bufs = []
        self.n_inst = 0

    def buf(self, dma=False, excl=False):
        b = Buf(excl)
        if dma:
            b.dsi = (self.dma_free_sw if dma == "sw" else self.dma_free).pop()
            self.phase_bufs.append(b)
        return b

    def bufs(self, n, dma=False, excl=False):
        return [self.buf(dma, excl) for _ in range(n)]

    @staticmethod
    def _split(reads, writes):
        ex = [b for b in reads if b.excl]
        if ex:
            return [b for b in reads if not b.excl], list(writes) + ex
        return reads, writes

    def _deps(self, reads, writes):
        deps = {}
        for b in reads:
            if b.w is not None:
                si, v = b.w
                if deps.get(si, 0) < v:
                    deps[si] = v
        for b in writes:
            if b.w is not None:
                si, v = b.w
                if deps.get(si, 0) < v:
                    deps[si] = v
            for si, v in b.r.items():
                if deps.get(si, 0) < v:
                    deps[si] = v
        return deps

    def _wait(self, e, deps):
        for si, v in deps.items():
            if e.seen.get(si, 0) >= v:
                continue
            if si == e.si and not e.selfsync:
                continue
            e.h.wait_ge(self.sems[si], v)
            e.seen[si] = v
            self.n_inst += 1

    def _mark(self, ev, reads, writes):
        si, v = ev
        for b in reads:
            b.r[si] = v
        for b in writes:
            b.w = ev
            b.r = {}

    def op(self, ename, fn, reads=(), writes=()):
        reads, writes = self._split(reads, writes)
        e = self.engs[ename]
        self._wait(e, self._deps(reads, writes))
        ins = fn(e.h)
        self.semval[e.si] += 1
        ins.then_inc(self.sems[e.si], 1)
        self.n_inst += 1
        self._mark((e.si, self.semval[e.si]), reads, writes)

    def dma(self, qname, out, in_, reads=(), writes=(), **kw):
        e = self.engs[qname]
        self._wait(e, self._deps(reads, writes))
        b0 = None
        for b in list(writes) + list(reads):
            if b.dsi is not None:
                b0 = b
                break
        assert b0 is not None
        assert (b0.dsi in self.sw_sems) == (qname == "pool"), "DMA queue / semaphore class mismatch"
        ins = e.h.dma_start(out=out, in_=in_, **kw)
        self.semval[b0.dsi] += 16
        ins.then_inc(self.sems[b0.dsi], 16)
        self.n_inst += 1
        self._mark((b0.dsi, self.semval[b0.dsi]), reads, writes)

    def barrier(self):
        sp = self.engs["sp"]
        for si in range(len(self.sems)):
            if si == self.bar_si or si == sp.si:
                continue
            v = self.semval[si]
            if v > 0 and sp.seen.get(si, 0) < v:
                sp.h.wait_ge(self.sems[si], v)
                sp.seen[si] = v
        self.semval[self.bar_si] += 1
        sp.h.sem_inc(self.sems[self.bar_si], 1)
        bv = self.semval[self.bar_si]
        for name, e in self.engs.items():
            if name != "sp":
                e.h.wait_ge(self.sems[self.bar_si], bv)
            for si in range(len(self.sems)):
                e.seen[si] = self.semval[si]
        for b in self.phase_bufs:
            (self.dma_free_sw if b.dsi in self.sw_sems else self.dma_free).append(b.dsi)
            b.dsi = None
        self.phase_bufs = []


def _rope_tables():
    inv = np.power(np.float32(10000.0), -(np.arange(0, 64, 2, dtype=np.float32) / np.float32(64))).astype(np.float32)
    ang = (np.arange(T, dtype=np.float32)[:, None] * inv[None, :]).astype(np.float32)
    cos = np.cos(ang).astype(np.float32)
    sin = np.sin(ang).astype(np.float32)
    cosT = np.zeros((128, T), np.float32)
    sinT = np.zeros((128, T), np.float32)
    for p in range(128):
        d = p % 64
        cosT[p] = cos[:, d % 32]
        sinT[p] = sin[:, d % 32] * (-1.0 if d < 32 else 1.0)
    return cosT, sinT


def _consts():
    c = {}
    bf = ml_dtypes.bfloat16
    c["c_ident"] = np.eye(128, dtype=np.float32).astype(bf)
    cosT, sinT = _rope_tables()
    c["c_cos"] = cosT
    c["c_sin"] = sinT
    P = np.zeros((128, 128), np.float32)
    for m in range(128):
        d = m % 64
        base = m - d
        if d < 32:
            P[base + d + 32, m] = 1.0
        else:
            P[base + d - 32, m] = 1.0
    c["c_rot"] = P
    B1 = np.zeros((128, 128), np.float32)
    B1[:64, :64] = 1.0
    B1[64:, 64:] = 1.0
    c["c_bones"] = B1.astype(bf)
    k = np.arange(128)[:, None]
    q = np.arange(128)[None, :]
    c["c_mdiag"] = (k <= q).astype(np.float32).astype(bf)
    c["c_mprev"] = (k > q).astype(np.float32).astype(bf)
    c["c_mdiagb"] = np.where(k <= q, 0.0, -30000.0).astype(np.float32).astype(bf)
    c["c_mprevb"] = np.where(k > q, 0.0, -30000.0).astype(np.float32).astype(bf)
    c["c_negdiag"] = np.where(q.T >= k.T, 0.0, NEG).astype(np.float32)
    qq = np.arange(128)[:, None]
    kk = np.arange(128)[None, :]
    c["c_negdiag"] = np.where(kk <= qq, 0.0, NEG).astype(np.float32)
    cidx = np.arange(256)
    tpos = np.arange(T)
    cm = ((16 * cidx[:, None] + 31) <= tpos[None, :]) & (cidx[:, None] < 255)
    c["c_mcmp"] = cm.reshape(2, 128, T).transpose(1, 0, 2).astype(np.float32).astype(bf).copy()
    j = np.arange(64)
    ov = ((16 * cidx[:, None] < 64 * j[None, :] + 64) & (16 * cidx[:, None] + 32 > 64 * j[None, :]) & (cidx[:, None] < 255))
    c["c_ovl"] = ov.reshape(2, 128, 64).transpose(1, 0, 2).astype(np.float32).astype(bf).copy()
    c["c_bsel"] = (np.arange(T)[None, :] // 64 == j[:, None]).astype(np.float32).astype(bf)
    cur = tpos // 64
    forced = (j[None, :] == 0) | (j[None, :] == cur[:, None]) | (j[None, :] == cur[:, None] - 1)
    noncausal = (64 * j[None, :]) > tpos[:, None]
    F1 = np.where(forced, 1e9, 0.0).astype(np.float32)
    F2 = np.where(noncausal, NEG, 3e38).astype(np.float32)
    gs = np.zeros((32, 24, 64), np.float32)
    for r_ in range(24):
        gs[r_, r_, :] = 1.0
    c["c_gsel"] = gs
    c["c_p2"] = np.tile((0.5 ** np.arange(1, 25, dtype=np.float64)).astype(np.float32)[None, :], (128, 1))
    c["c_f1"] = F1.reshape(NT, 128, 64).transpose(1, 0, 2).copy()
    c["c_f2"] = F2.reshape(NT, 128, 64).transpose(1, 0, 2).copy()
    return c


CONST_SPECS = None


def build_program(depth=DEPTH, debug=False, stop_after=None):
    nc = bass.Bass("TRN2", target_bir_lowering=False)
    consts = _consts()

    def dram_in(name, shape, dt=F32):
        return nc.dram_tensor(name, list(shape), dt, kind="ExternalInput").ap()

    scratch_kind = "ExternalOutput" if debug else "Internal"

    def dram_scr(name, shape, dt):
        return nc.dram_tensor(name, list(shape), dt, kind=scratch_kind).ap()

    x_in = dram_in("x", [T, D])
    norm_mix = dram_in("norm_mix", [DEPTH, D])
    w_in = dram_in("w_in", [DEPTH, D, D_IN])
    gains = dram_in("gains", [DEPTH, 128, 6])
    sinks = dram_in("sinks", [DEPTH, 8])
    peT = dram_in("peT", [DEPTH, 2, 128, 32])
    w_ck1 = dram_in("w_ck1", [DEPTH, 2048, 256])
    w_ck2 = dram_in("w_ck2", [DEPTH, 256, 64])
    w_cv1 = dram_in("w_cv1", [DEPTH, 2048, 256])
    w_cv2 = dram_in("w_cv2", [DEPTH, 256, 64])
    w_branch = dram_in("w_branch", [DEPTH, 3, 512, D])
    w_out = dram_in("w_out", [DEPTH, D, D])
    norm_mlp = dram_in("norm_mlp", [DEPTH, D])
    w_up = dram_in("w_up", [DEPTH, D, D_FF])
    w_down = dram_in("w_down", [DEPTH, D_FF, D])
    cin = {}
    for k, v in consts.items():
        cin[k] = dram_in(k, v.shape, BF16 if v.dtype == ml_dtypes.bfloat16 else F32)
    y_out = nc.dram_tensor("y", [T, D], F32, kind="ExternalOutput").ap()

    xs = [dram_scr("xs0", [T, D], F32), dram_scr("xs1", [T, D], F32)]
    QT = [dram_scr("QT%d" % m, [128, 4, T], BF16) for m in range(3)]
    KT = [dram_scr("KT%d" % m, [128, T], BF16) for m in range(5)]
    VcT = dram_scr("VcT", [128, T], BF16)
    IqT = dram_scr("IqT", [128, 2, T], BF16)
    IkT = dram_scr("IkT", [128, T], BF16)
    sgT = dram_scr("sgT", [128, 24, T], BF16)
    Vtok = dram_scr("Vtok", [T, 512], BF16)
    gciw = dram_scr("gciw", [T, 32], F32)
    OT = [dram_scr("OT%d" % m, [64, 8, T], BF16) for m in range(3)]
    KcmpT = dram_scr("KcmpT", [128, 256], BF16)
    Vcmp = dram_scr("Vcmp", [128, 2, 2, 64], BF16)
    sgcT2 = dram_scr("sgcT2", [32, T], F32)

    with ExitStack() as es:
        kb = KB(nc, es)
        for l in range(depth):
            x_src = x_in if l == 0 else xs[1]
            x_dst = y_out if l == depth - 1 else xs[1]
            phase_proj(nc, kb, l, x_src, norm_mix, w_in, gains, cin, QT, KT, VcT, IqT, IkT, sgT, Vtok, gciw, sgcT2, stop=stop_after)
            kb.barrier()
            if stop_after in ("proj", "norm"):
                break
            if DBG.get("skip_a") is None:
                phase_dsa(nc, kb, l, cin, QT[0], KT[0], IqT, IkT, Vtok, gciw, OT[0], qtiles=DBG.get("qtiles"))
            if stop_after == "dsa":
                break
            phase_swa(nc, kb, l, cin, sinks, QT[1], KT[1], Vtok, OT[1], qtiles=DBG.get("qtiles"))
            if stop_after == "swa":
                break
            phase_cmp(nc, kb, l, KT[2], VcT, peT, w_ck1, w_ck2, w_cv1, w_cv2, KcmpT, Vcmp)
            phase_nsa(nc, kb, l, cin, QT[2], KT[3], KT[4], Vtok, KcmpT, Vcmp, sgcT2, OT[2], qtiles=DBG.get("qtiles"))
            if stop_after == "nsa":
                break
            phase_merge(nc, kb, l, x_src, xs[0], OT, sgT, w_branch, w_out)
            if stop_after == "merge":
                break
            phase_mlp(nc, kb, l, xs[0], x_dst, norm_mlp, w_up, w_down, cin)
        kb.barrier()
    return nc, consts


def phase_proj(nc, kb, l, x_src, norm_mix, w_in, gains, cin, QT, KT, VcT, IqT, IkT, sgT, Vtok, gciw, sgcT2, stop=None):
    with ExitStack() as es:
        def sb(name, shape, dt):
            return es.enter_context(nc.sbuf_tensor(_uniq(name), list(shape), dt))

        def ps(name, shape, dt):
            return es.enter_context(nc.psum_tensor(_uniq(name), list(shape), dt))

        hT = sb("hT", [128, 8, T], BF16)
        hT_b = kb.bufs(8)
        gt = sb("gt", [128, D], F32)
        gt_b = kb.buf(dma=True)
        ident = sb("ident", [128, 128], BF16)
        ident_b = kb.buf(dma=True)
        kb.dma("sp", gt[:], norm_mix[l].partition_broadcast(128), writes=[gt_b])
        kb.dma("sp", ident[:], cin["c_ident"], writes=[ident_b])

        xt = [sb("xt%d" % i, [128, D], F32) for i in range(2)]
        xt_b = kb.bufs(2, dma=True)
        junk = sb("junk", [128, D], F32)
        junk_b = kb.buf()
        st = [sb("st%d" % i, [128, 4], F32) for i in range(2)]
        st_b = kb.bufs(2)
        hb = [sb("hb%d" % i, [128, D], BF16) for i in range(2)]
        hb_b = kb.bufs(2)
        pst = [ps("pst%d" % i, [128, 8, 128], BF16) for i in range(2)]
        pst_b = kb.bufs(2, excl=True)
        for tt in range(NT):
            i = tt % 2
            kb.dma("sp", xt[i][:], x_src[tt * 128:(tt + 1) * 128, :], writes=[xt_b[i]])
            kb.op("act", lambda e: e.activation(out=junk[:], in_=xt[i][:], func=AF.Square, accum_out=st[i][:, 0:1]),
                  reads=[xt_b[i]], writes=[junk_b, st_b[i]])
            kb.op("act", lambda e: e.activation(out=st[i][:, 1:2], in_=st[i][:, 0:1], func=AF.Sqrt, bias=EPS, scale=1.0 / D),
                  reads=[st_b[i]], writes=[st_b[i]])
            kb.op("dve", lambda e: e.reciprocal(out=st[i][:, 2:3], in_=st[i][:, 1:2]), reads=[st_b[i]], writes=[st_b[i]])
            kb.op("dve", lambda e: e.scalar_tensor_tensor(out=hb[i][:], in0=xt[i][:], scalar=st[i][:, 2:3], in1=gt[:],
                                                          op0=ALU.mult, op1=ALU.mult),
                  reads=[xt_b[i], st_b[i], gt_b], writes=[hb_b[i]])
            for kc in range(8):
                kb.op("pe", lambda e: e.transpose(out=pst[i][:, kc, :], in_=hb[i][:, kc * 128:(kc + 1) * 128], identity=ident[:]),
                      reads=[hb_b[i], ident_b], writes=[pst_b[i]])
            eng = "act" if tt % 2 == 0 else "pool"
            if eng == "pool":
                eng = "dve"
            kb.op(eng, lambda e: e.tensor_copy(out=hT[:, :, tt * 128:(tt + 1) * 128], in_=pst[i][:]) if eng != "act"
                  else e.activation(out=hT[:, :, tt * 128:(tt + 1) * 128], in_=pst[i][:], func=AF.Copy),
                  reads=[pst_b[i]], writes=[hT_b[tt // 4]])

        if stop == 'norm':
            dbg = sb('dbg', [128, 512], BF16); dbg_b = kb.buf(dma=True)
            kb.op('dve', lambda e: e.tensor_copy(out=dbg[:], in_=hT[:, 0, 0:512]), reads=hT_b, writes=[dbg_b])
            kb.dma('sp', KT[0][:, 0:512], dbg[:], reads=[dbg_b])
            kb.barrier()
            return
        cosT = sb("cosT", [128, T], F32)
        sinT = sb("sinT", [128, T], F32)
        cs_b = kb.buf(dma=True)
        kb.dma("sp", cosT[:], cin["c_cos"], writes=[cs_b])
        sn_b = kb.buf(dma=True)
        kb.dma("sp", sinT[:], cin["c_sin"], writes=[sn_b])
        rotc = sb("rotc", [128, 128], F32)
        rotc_b = kb.buf(dma=True)
        kb.dma("sp", rotc[:], cin["c_rot"], writes=[rotc_b])
        rot1 = sb("rot1", [128, 128], BF16)
        rot1_b = kb.buf()
        kb.op("dve", lambda e: e.tensor_copy(out=rot1[:], in_=rotc[:]), reads=[rotc_b], writes=[rot1_b])
        bones = sb("bones", [128, 128], BF16)
        bones_b = kb.buf(dma=True)
        kb.dma("sp", bones[:], cin["c_bones"], writes=[bones_b])
        gn = sb("gn", [128, 6], F32)
        gn_b = kb.buf(dma=True)
        kb.dma("sp", gn[:], gains[l], writes=[gn_b])
        rotg = sb("rotg", [128, 6, 128], BF16)
        rotg_b = kb.buf()
        for gi in range(6):
            kb.op("dve", lambda e: e.tensor_scalar(out=rotg[:, gi, :], in0=rotc[:], scalar1=gn[:, gi:gi + 1], scalar2=None, op0=ALU.mult),
                  reads=[rotc_b, gn_b], writes=[rotg_b])

        chunks = []
        for m, qn in enumerate(["qa", "qb", "qc"]):
            for g in range(4):
                chunks.append(("qk", m, QT[m][:, g, :], [(0, OFF[qn] + g * 64, 64), (64, OFF[qn] + (4 + g) * 64, 64)]))
        for ki, (kn, m) in enumerate([("ka", 0), ("kb", 1), ("kc", 2), ("ks", 2), ("kw", 2)]):
            chunks.append(("qk", 3 + m, KT[ki], [(0, OFF[kn], 128)]))
        chunks.append(("copy", None, VcT, [(0, OFF["vc"], 128)]))
        for c in range(2):
            chunks.append(("rope", None, IqT[:, c, :], [(0, OFF["iq"] + c * 128, 128)]))
        chunks.append(("rope", None, IkT, [(0, OFF["ik"], 64), (64, OFF["ik"], 64)]))
        for c in range(24):
            chunks.append(("sig", None, sgT[:, c, :], [(0, OFF["gm"] + c * 128, 128)]))
        chunks.append(("sigf", None, sgcT2, [(0, OFF["gc"], 24)]))

        wc = [sb("wc%d" % i, [128, 8, 128], BF16) for i in range(2)]
        wc_b = kb.bufs(2, dma="sw")
        psz = [ps("psz%d" % i, [128, 512], F32) for i in range(2)]
        psz_b = kb.bufs(2, excl=True)
        pss = ps("pss", [128, 512], F32)
        pss_b = kb.buf(excl=True)
        psr = ps("psr", [128, 512], F32)
        psr_b = kb.buf(excl=True)
        sq = sb("sq", [128, 512], BF16); sq_b = kb.buf()
        zb = sb("zb", [128, 512], BF16); zb_b = kb.buf()
        rs = sb("rs", [128, 512], F32); rs_b = kb.buf()
        rinv = sb("rinv", [128, 512], F32); rinv_b = kb.buf()
        t1 = sb("t1", [128, 512], F32); t1_b = kb.buf()
        t2 = sb("t2", [128, 512], F32); t2_b = kb.buf()
        t3 = sb("t3", [128, 512], F32); t3_b = kb.buf()
        og = [sb("og%d" % i, [128, 512], BF16) for i in range(2)]
        og_b = kb.bufs(2, dma=True)
        ogf = sb("ogf", [32, 512], F32)
        ogf_b = kb.buf(dma=True)
        w_in_l = w_in[l]
        blk = 0
        if DBG.get('chunks') is not None:
            chunks = [chunks[i] for i in DBG['chunks']]
        sq2 = [sq, sb("sq_b", [128, 512], BF16)]; sq2_b = [sq_b, kb.buf()]
        zb2 = [zb, sb("zb_b", [128, 512], BF16)]; zb2_b = [zb_b, kb.buf()]
        ppipe = Pipe(1)
        for ci, (kind, gi, dst, pieces) in enumerate(chunks):
            wi = ci % 2
            for (poff, c0, n) in pieces:
                kb.dma("pool", wc[wi][:, :, poff:poff + n],
                       w_in_l[:, c0:c0 + n].rearrange("(kc p) n -> p kc n", p=128), writes=[wc_b[wi]])
            for t4 in range(8):
                zi = blk % 2
                oi = blk % 2
                blk += 1
                tsl = slice(t4 * 512, (t4 + 1) * 512)

                def front(kind=kind, gi=gi, wi=wi, t4=t4, zi=zi, tsl=tsl):
                    for kc in range(8):
                        kb.op("pe", lambda e: e.matmul(psz[zi][:], lhsT=wc[wi][:, kc, :], rhs=hT[:, kc, tsl], start=(kc == 0), stop=(kc == 7)),
                              reads=[wc_b[wi], hT_b[t4]], writes=[psz_b[zi]])
                    if kind in ("qk", "rope"):
                        kb.op("act", lambda e: e.activation(out=zb2[zi][:], in_=psz[zi][:], func=AF.Copy), reads=[psz_b[zi]], writes=[zb2_b[zi]])
                    if kind == "qk":
                        kb.op("act", lambda e: e.activation(out=sq2[zi][:], in_=psz[zi][:], func=AF.Square), reads=[psz_b[zi]], writes=[sq2_b[zi]])

                def back(kind=kind, gi=gi, dst=dst, zi=zi, oi=oi, tsl=tsl):
                    zb, zb_b, sq, sq_b = zb2[zi], zb2_b[zi], sq2[zi], sq2_b[zi]
                    if kind == "sig":
                        kb.op("act", lambda e: e.activation(out=og[oi][:], in_=psz[zi][:], func=AF.Sigmoid), reads=[psz_b[zi]], writes=[og_b[oi]])
                    elif kind == "sigf":
                        kb.op("act", lambda e: e.activation(out=ogf[:], in_=psz[zi][0:32, :], func=AF.Sigmoid), reads=[psz_b[zi]], writes=[ogf_b])
                        kb.dma("sp", dst[:, tsl], ogf[:], reads=[ogf_b])
                        return
                    elif kind == "copy":
                        kb.op("act", lambda e: e.activation(out=og[oi][:], in_=psz[zi][:], func=AF.Copy), reads=[psz_b[zi]], writes=[og_b[oi]])
                    else:
                        if kind == "qk":
                            kb.op("pe", lambda e: e.matmul(pss[:], lhsT=bones[:], rhs=sq[:], start=True, stop=True),
                                  reads=[bones_b, sq_b], writes=[pss_b])
                            kb.op("pe", lambda e: e.matmul(psr[:], lhsT=rotg[:, gi, :], rhs=zb[:], start=True, stop=True),
                                  reads=[rotg_b, zb_b], writes=[psr_b])
                            kb.op("act", lambda e: e.activation(out=rs[:], in_=pss[:], func=AF.Ln, bias=EPS, scale=1.0 / 64),
                                  reads=[pss_b], writes=[rs_b])
                            kb.op("act", lambda e: e.activation(out=rinv[:], in_=rs[:], func=AF.Exp, scale=-0.5), reads=[rs_b], writes=[rinv_b])
                            kb.op("dve", lambda e: e.scalar_tensor_tensor(out=t1[:], in0=psz[zi][:], scalar=gn[:, gi:gi + 1], in1=cosT[:, tsl],
                                                                          op0=ALU.mult, op1=ALU.mult),
                                  reads=[psz_b[zi], gn_b, cs_b], writes=[t1_b])
                        else:
                            kb.op("pe", lambda e: e.matmul(psr[:], lhsT=rot1[:], rhs=zb[:], start=True, stop=True),
                                  reads=[rot1_b, zb_b], writes=[psr_b])
                            kb.op("dve", lambda e: e.tensor_tensor(out=t1[:], in0=psz[zi][:], in1=cosT[:, tsl], op=ALU.mult),
                                  reads=[psz_b[zi], cs_b], writes=[t1_b])
                        kb.op("dve", lambda e: e.tensor_tensor(out=t2[:], in0=psr[:], in1=sinT[:, tsl], op=ALU.mult),
                              reads=[psr_b, sn_b], writes=[t2_b])
                        if kind == "qk":
                            kb.op("pool", lambda e: e.tensor_tensor(out=t3[:], in0=t1[:], in1=t2[:], op=ALU.add), reads=[t1_b, t2_b], writes=[t3_b])
                            kb.op("pool", lambda e: e.tensor_tensor(out=og[oi][:], in0=t3[:], in1=rinv[:], op=ALU.mult),
                                  reads=[t3_b, rinv_b], writes=[og_b[oi]])
                        else:
                            kb.op("pool", lambda e: e.tensor_tensor(out=og[oi][:], in0=t1[:], in1=t2[:], op=ALU.add), reads=[t1_b, t2_b], writes=[og_b[oi]])
                    kb.dma("sp", dst[:, tsl], og[oi][:], reads=[og_b[oi]])

                ppipe.push(front, back)
        ppipe.flush()

        if DBG.get('notok'):
            kb.barrier()
            return
        wv = sb("wv", [128, 8, 512], BF16)
        wv_b = kb.buf(dma="sw")
        for vi, vn in enumerate(["va", "vb", "vs", "vw"]):
            kb.dma("pool", wv[:, :, vi * 128:(vi + 1) * 128],
                   w_in_l[:, OFF[vn]:OFF[vn] + 128].rearrange("(kc p) n -> p kc n", p=128), writes=[wv_b])
        wg = sb("wg", [128, 8, 32], BF16)
        wg_b = kb.buf(dma="sw")
        kb.op("dve", lambda e: e.memset(wg[:], 0.0), writes=[wg_b])
        kb.dma("pool", wg[:, :, 0:24], w_in_l[:, OFF["gc"]:OFF["gc"] + 24].rearrange("(kc p) n -> p kc n", p=128), writes=[wg_b])
        kb.dma("pool", wg[:, :, 24:28], w_in_l[:, OFF["iw"]:OFF["iw"] + 4].rearrange("(kc p) n -> p kc n", p=128), writes=[wg_b])
        psg = [ps("psg%d" % i, [128, 512], F32) for i in range(2)]
        psg_b = kb.bufs(2, excl=True)
        ov = [sb("ov%d" % i, [128, 512], BF16) for i in range(2)]
        ov_b = kb.bufs(2, dma=True)
        ogc = [sb("ogc%d" % i, [128, 32], F32) for i in range(2)]
        ogc_b = kb.bufs(2, dma=True)
        for tt in range(NT):
            i = tt % 2
            tsl = slice(tt * 128, (tt + 1) * 128)
            for kc in range(8):
                kb.op("pe", lambda e: e.matmul(psz[i][:], lhsT=hT[:, kc, tsl], rhs=wv[:, kc, :], start=(kc == 0), stop=(kc == 7)),
                      reads=[wv_b, hT_b[tt // 4]], writes=[psz_b[i]])
            for kc in range(8):
                kb.op("pe", lambda e: e.matmul(psg[i][:, 0:32], lhsT=hT[:, kc, tsl], rhs=wg[:, kc, :], start=(kc == 0), stop=(kc == 7)),
                      reads=[wg_b, hT_b[tt // 4]], writes=[psg_b[i]])
            kb.op("act", lambda e: e.activation(out=ov[i][:], in_=psz[i][:], func=AF.Copy), reads=[psz_b[i]], writes=[ov_b[i]])
            kb.op("dve", lambda e: e.tensor_copy(out=ogc[i][:], in_=psg[i][:, 0:32]), reads=[psg_b[i]], writes=[ogc_b[i]])
            kb.dma("sp", Vtok[tsl, :], ov[i][:], reads=[ov_b[i]])
            kb.dma("sp", gciw[tsl, :], ogc[i][:], reads=[ogc_b[i]])
        kb.barrier()


def _host_inputs(inputs, consts):
    per_core = []
    qn = np.asarray(inputs["q_norm"], np.float32)
    kn = np.asarray(inputs["k_norm"], np.float32)
    gains = np.zeros((DEPTH, 128, 6), np.float32)
    for l in range(DEPTH):
        for m in range(3):
            gains[l, :, m] = np.tile(qn[l, m], 2)
            gains[l, :, 3 + m] = np.tile(kn[l, m], 2)
    pe = np.stack([np.asarray(inputs["cmp_pe_k"], np.float32), np.asarray(inputs["cmp_pe_v"], np.float32)], axis=1)
    peT = np.ascontiguousarray(np.tile(pe.transpose(0, 1, 3, 2), (1, 1, 2, 1)))
    shared = {
        "norm_mix": np.asarray(inputs["norm_mix"], np.float32),
        "w_in": np.asarray(inputs["w_in"], np.float32),
        "gains": gains,
        "sinks": np.asarray(inputs["sinks"], np.float32),
        "peT": peT,
        "w_ck1": np.asarray(inputs["w_ck1"], np.float32),
        "w_ck2": np.asarray(inputs["w_ck2"], np.float32),
        "w_cv1": np.asarray(inputs["w_cv1"], np.float32),
        "w_cv2": np.asarray(inputs["w_cv2"], np.float32),
        "w_branch": np.asarray(inputs["w_branch"], np.float32),
        "w_out": np.asarray(inputs["w_out"], np.float32),
        "norm_mlp": np.asarray(inputs["norm_mlp"], np.float32),
        "w_up": np.asarray(inputs["w_up"], np.float32),
        "w_down": np.asarray(inputs["w_down"], np.float32),
    }
    shared.update(consts)
    x = np.asarray(inputs["x"], np.float32)
    for c in range(N_CORES):
        m = dict(shared)
        m["x"] = np.ascontiguousarray(x[c % 4])
        per_core.append(m)
    return per_core


def kernel(**inputs):
    nc, consts = build_program()
    in_maps = _host_inputs(inputs, consts)
    res = run_bass_kernel_spmd(nc, in_maps, core_ids=list(range(N_CORES)))
    out = np.stack([np.asarray(res.results[c]["y"], np.float32) for c in range(4)], axis=0)
    return out


class _Scope:
    def __init__(self, nc, es):
        self.nc, self.es = nc, es

    def sb(self, name, shape, dt):
        return self.es.enter_context(self.nc.sbuf_tensor(_uniq(name), list(shape), dt))

    def ps(self, name, shape=(128, 512), dt=F32):
        return self.es.enter_context(self.nc.psum_tensor(_uniq(name), list(shape), dt))


def _load_qkv(nc, kb, S, tag, QTd, KTd, Vtok, vi):
    q = S.sb("q_" + tag, [128, 4, T], BF16)
    q_b = kb.buf(dma=True)
    for g in range(4):
        kb.dma("sp", q[:, g, :], QTd[:, g, :], writes=[q_b])
    k = S.sb("k_" + tag, [128, T], BF16)
    k_b = kb.buf(dma=True)
    kb.dma("sp", k[:], KTd, writes=[k_b])
    v = S.sb("v_" + tag, [128, NT, 2, 65], BF16)
    v_b = kb.buf(dma=True)
    kb.op("pool", lambda e: e.memset(v[:], 1.0), writes=[v_b])
    for hk in range(2):
        kb.dma("sp", v[:, :, hk, 0:64],
               Vtok[:, vi * 128 + hk * 64: vi * 128 + hk * 64 + 64].rearrange("(kt p) d -> p kt d", p=128), writes=[v_b])
    return (q, q_b), (k, k_b), (v, v_b)


class AttnRes:
    def __init__(self, nc, kb, S, out_dt=BF16):
        self.psS = [S.ps("psS%d" % i) for i in range(2)]
        self.psS_b = kb.bufs(2, excl=True)
        self.psO = [S.ps("psO%d" % i) for i in range(2)]
        self.psO_b = kb.bufs(2, excl=True)
        self.psD = S.ps("psD")
        self.psD_b = kb.buf(excl=True)
        self.E = [S.sb("E%d" % i, [128, 512], BF16) for i in range(4)]
        self.E_b = kb.bufs(4)
        self.pipe = Pipe(DBG.get("look", 2))
        self.oTs2 = [S.sb("oTs%d" % i, [65, 512], F32) for i in range(2)]
        self.oTs2_b = kb.bufs(2)
        self.rr2 = [S.sb("rr%d" % i, [65, 512], F32) for i in range(2)]
        self.rr2_b = kb.bufs(2)
        self.oTs, self.oTs_b, self.rr, self.rr_b = self.oTs2[0], self.oTs2_b[0], self.rr2[0], self.rr2_b[0]
        self.onb = [S.sb("onb%d" % i, [64, 512], out_dt) for i in range(2)]
        self.onb_b = kb.bufs(2, dma=True)
        self.ones32 = S.sb("ones32", [65, 64], F32)
        self.ones32_b = kb.buf()
        kb.op("dve", lambda e: e.memset(self.ones32[:], 1.0), writes=[self.ones32_b])
        self.cnt = 0
        self.pcnt = 0
        self.ocnt = 0


class Pipe:
    def __init__(self, look=2):
        self.look = look
        self.pending = []
        self.bg = []
        self.quota = 0
        self.burst = 0
        self.nburst = 0

    def set_bg(self, units, nsteps, burst=0, nburst=0):
        self.bg = list(units)
        self.burst, self.nburst = burst, nburst
        rest = max(0, len(self.bg) - nburst)
        bsteps = -(-nburst // burst) if burst else 0
        self.quota = -(-rest // max(1, nsteps - bsteps))
        if DBG.get("nobg"):
            self.flush_bg()

    def push(self, front, back):
        front()
        self.pending.append(back)
        while len(self.pending) > self.look:
            self.pending.pop(0)()
        if self.nburst > 0:
            for _ in range(self.burst):
                if self.bg and self.nburst > 0:
                    self.bg.pop(0)()
                    self.nburst -= 1
            return
        for _ in range(self.quota):
            if self.bg:
                self.bg.pop(0)()

    def flush(self):
        while self.pending:
            self.pending.pop(0)()

    def flush_bg(self):
        while self.bg:
            self.bg.pop(0)()


def attn_tile(kb, R, hk, qsl, klist, K, Q, V, maskfn, extra_den=None, mask_eng=None, tiny=None, post=None, st_override=None):
    mask_eng = mask_eng or DBG.get("mask_eng", "pool")
    (k, k_b), (q, q_b), (v, v_b) = K, Q, V
    hs = slice(hk * 64, (hk + 1) * 64)
    nk = len(klist)
    NB = len(R.E)

    def finalize():
        oi = R.ocnt % 2
        R.ocnt += 1
        oTs, oTs_b, rr, rr_b = R.oTs2[oi], R.oTs2_b[oi], R.rr2[oi], R.rr2_b[oi]
        kb.op("act", lambda e: e.activation(out=oTs[0:65, :], in_=R.psO[hk][0:65, :], func=AF.Copy), reads=[R.psO_b[hk]], writes=[oTs_b])
        if extra_den is not None:
            ed_ap, ed_b = extra_den
            kb.op("dve", lambda e: e.tensor_tensor(out=oTs[64:65, :], in0=oTs[64:65, :], in1=ed_ap, op=ALU.add),
                  reads=[oTs_b, ed_b], writes=[oTs_b])
        if tiny is not None:
            kb.op("dve", lambda e: e.tensor_scalar(out=oTs[64:65, :], in0=oTs[64:65, :], scalar1=tiny, scalar2=None, op0=ALU.max),
                  reads=[oTs_b], writes=[oTs_b])
        kb.op("act", lambda e: e.activation(out=rr[64:65, :], in_=oTs[64:65, :], func=AF.Ln), reads=[oTs_b], writes=[rr_b])
        kb.op("act", lambda e: e.activation(out=rr[64:65, :], in_=rr[64:65, :], func=AF.Exp, scale=-1.0), reads=[rr_b], writes=[rr_b])
        kb.op("pe", lambda e: e.matmul(R.psD[0:64, :], lhsT=R.ones32[64:65, 0:64], rhs=rr[64:65, :], start=True, stop=True),
              reads=[R.ones32_b, rr_b], writes=[R.psD_b])
        kb.op("dve", lambda e: e.tensor_tensor(out=R.onb[oi][:], in0=oTs[0:64, :], in1=R.psD[0:64, :], op=ALU.mult),
              reads=[oTs_b, R.psD_b], writes=[R.onb_b[oi]])
        post(R.onb[oi], R.onb_b[oi])

    for idx, j in enumerate(klist):
        def front(idx=idx, j=j):
            pi = R.pcnt % 2
            R.pcnt += 1
            si = R.cnt % NB
            R.cnt += 1
            ksl = slice(j * 128, (j + 1) * 128)
            ms = maskfn(j) or []
            psv = R.psS[pi][:].rearrange("p (g q) -> p g q", g=4)
            if st_override is not None:
                l_ap0, r_ap0, bufs0 = st_override(j)
                kb.op("pe", lambda e: e.matmul(psv, lhsT=l_ap0, rhs=r_ap0, start=True, stop=(len(ms) == 0)),
                      reads=list(bufs0), writes=[R.psS_b[pi]])
            else:
                kb.op("pe", lambda e: e.matmul(psv, lhsT=k[hs, ksl], rhs=q[hs, :, qsl], start=True, stop=(len(ms) == 0)),
                      reads=[k_b, q_b], writes=[R.psS_b[pi]])
            for mi, (l_ap, r_ap, bufs) in enumerate(ms):
                kb.op("pe", lambda e: e.matmul(psv, lhsT=l_ap, rhs=r_ap.unsqueeze(1).to_broadcast([r_ap.shape[0], 4, 128]),
                                               start=False, stop=(mi == len(ms) - 1)),
                      reads=list(bufs), writes=[R.psS_b[pi]])
            kb.op("act", lambda e: e.activation(out=R.E[si][:], in_=R.psS[pi][:], func=AF.Exp, scale=0.125),
                  reads=[R.psS_b[pi]], writes=[R.E_b[si]])
            return si

        def back(idx=idx, j=j, si_box=None):
            pass

        box = {}

        def front2(front=front, box=box):
            box["si"] = front()

        def back2(idx=idx, j=j, box=box):
            si = box["si"]
            kb.op("pe", lambda e: e.matmul(R.psO[hk][0:65, :], lhsT=v[:, j, hk, :], rhs=R.E[si][:], start=(idx == 0), stop=(idx == nk - 1)),
                  reads=[v_b, R.E_b[si]], writes=[R.psO_b[hk]])
            if idx == nk - 1:
                finalize()

        R.pipe.push(front2, back2)


NIT = 20
MASKB = 30000.0
BIS_DVE_FRAC = 0.5
IDX_ENG = "dve"


def phase_dsa(nc, kb, l, cin, QTd, KTd, IqTd, IkTd, Vtok, gciw, OTd, qtiles=None):
    with ExitStack() as es:
        S = _Scope(nc, es)
        Q, K, V = _load_qkv(nc, kb, S, "a", QTd, KTd, Vtok, 0)
        iq = S.sb("iq", [128, 2, T], BF16); iq_b = kb.buf(dma=True)
        for c in range(2):
            kb.dma("sp", iq[:, c, :], IqTd[:, c, :], writes=[iq_b])
        ik = S.sb("ik", [128, T], BF16); ik_b = kb.buf(dma=True)
        kb.dma("sp", ik[:], IkTd, writes=[ik_b])
        iw = S.sb("iw", [128, NT, 4], F32); iw_b = kb.buf(dma=True)
        kb.dma("sp", iw[:], gciw[:, 24:28].rearrange("(kt p) d -> p kt d", p=128), writes=[iw_b])
        ident = S.sb("ident", [128, 128], BF16); ident_b = kb.buf(dma=True)
        kb.dma("sp", ident[:], cin["c_ident"], writes=[ident_b])
        negd = S.sb("negd", [128, 128], F32); negd_b = kb.buf(dma=True)
        kb.dma("sp", negd[:], cin["c_negdiag"], writes=[negd_b])
        p2 = S.sb("p2", [128, 24], F32); p2_b = kb.buf(dma=True)
        kb.dma("sp", p2[:], cin["c_p2"], writes=[p2_b])
        R = AttnRes(nc, kb, S)
        psI = [S.ps("psI%d" % i) for i in range(2)]
        psI_b = kb.bufs(2, excl=True)
        psT = S.ps("psT", [128, 8, 128], BF16)
        psT_b = kb.buf(excl=True)
        score = S.sb("score", [128, T], F32); score_b = kb.buf()
        rl = [S.sb("rl%d" % i, [128, 512], F32) for i in range(2)]
        rl_b = kb.bufs(2)
        junk = S.sb("junkc", [128, T], BF16); junk_b = kb.buf()
        maskb = S.sb("maskb", [128, T], BF16); maskb_b = kb.buf()
        maskT = S.sb("maskT", [128, NT, 128], BF16); maskT_b = kb.buf()
        stt = S.sb("stt", [128, 8], F32); stt_b = kb.buf()
        sta = S.sb("sta", [128, 2], F32); sta_b = kb.buf()
        midt = S.sb("midt", [128, 2], F32); mid_b = kb.buf()
        junka = S.sb("junka", [128, T], BF16); junka_b = kb.buf()
        wt = S.sb("wt", [128, 24], F32); wt_b = kb.buf()
        icnt = [0]
        maskT2 = [maskT, S.sb("maskTb", [128, NT, 128], BF16)]
        maskT2_b = [maskT_b, kb.buf()]

        def mask_units(i, mb):
            n = (i + 1) * 128
            qsl = slice(i * 128, n)
            units = []

            def idx_unit(k0, w, h):
                def u():
                    pi = icnt[0] % 2
                    icnt[0] += 1
                    hh = slice((h % 2) * 64, (h % 2) * 64 + 64)
                    kb.op("pe", lambda e: e.matmul(psI[pi][:, 0:w], lhsT=iq[hh, h // 2, qsl], rhs=ik[hh, k0:k0 + w], start=True, stop=True),
                          reads=[iq_b, ik_b], writes=[psI_b[pi]])
                    kb.op("act", lambda e: e.activation(out=rl[pi][:, 0:w], in_=psI[pi][:, 0:w], func=AF.Relu, scale=0.0625),
                          reads=[psI_b[pi]], writes=[rl_b[pi]])
                    if h == 0:
                        kb.op(IDX_ENG, lambda e: e.tensor_scalar(out=score[:, k0:k0 + w], in0=rl[pi][:, 0:w], scalar1=iw[:, i, 0:1], scalar2=None, op0=ALU.mult),
                              reads=[rl_b[pi], iw_b], writes=[score_b])
                    else:
                        kb.op("dve", lambda e: e.scalar_tensor_tensor(out=score[:, k0:k0 + w], in0=rl[pi][:, 0:w], scalar=iw[:, i, h:h + 1],
                                                                      in1=score[:, k0:k0 + w], op0=ALU.mult, op1=ALU.add),
                              reads=[rl_b[pi], iw_b, score_b], writes=[score_b])
                return u

            for k0 in range(0, n, 512):
                w = min(512, n - k0)
                for h in range(4):
                    units.append(idx_unit(k0, w, h))

            def init_unit():
                kb.op("dve", lambda e: e.tensor_tensor(out=score[:, i * 128:n], in0=score[:, i * 128:n], in1=negd[:], op=ALU.add),
                      reads=[score_b, negd_b], writes=[score_b])
                if i < 2:
                    kb.op("dve", lambda e: e.memset(midt[:, 0:1], -1e29), writes=[mid_b])
                else:
                    kb.op("dve", lambda e: e.tensor_reduce(out=stt[:, 0:1], in_=score[:, 0:n], axis=AX.X, op=ALU.max), reads=[score_b], writes=[stt_b])
                    kb.op("dve", lambda e: e.tensor_reduce(out=stt[:, 1:2], in_=score[:, 0:i * 128], axis=AX.X, op=ALU.min), reads=[score_b], writes=[stt_b])
                    kb.op("dve", lambda e: e.tensor_scalar(out=stt[:, 1:2], in0=stt[:, 1:2], scalar1=-1.0, scalar2=None, op0=ALU.add), reads=[stt_b], writes=[stt_b])
                    kb.op("dve", lambda e: e.tensor_tensor(out=stt[:, 2:3], in0=stt[:, 0:1], in1=stt[:, 1:2], op=ALU.subtract), reads=[stt_b], writes=[stt_b])
                    kb.op("dve", lambda e: e.tensor_scalar(out=wt[:], in0=p2[:], scalar1=stt[:, 2:3], scalar2=None, op0=ALU.mult), reads=[stt_b, p2_b], writes=[wt_b])
                    kb.op("dve", lambda e: e.tensor_tensor(out=midt[:, 0:1], in0=stt[:, 1:2], in1=wt[:, 0:1], op=ALU.add), reads=[stt_b, wt_b], writes=[mid_b])
            units.append(init_unit)

            def bis_unit(it):
                n_d = int((n * BIS_DVE_FRAC) // 128) * 128 if DBG.get("bis_split", True) else n
                m_a = n - n_d

                def u():
                    if m_a > 0:
                        kb.op("act", lambda e: e.activation(out=junka[:, 0:m_a], in_=score[:, n_d:n], func=AF.Sign, scale=-1.0, bias=midt[:, 0:1],
                                                            accum_out=sta[:, 0:1]),
                              reads=[score_b, mid_b], writes=[junka_b, sta_b])
                    kb.op("dve", lambda e: e.tensor_scalar(out=junk[:, 0:n_d], in0=score[:, 0:n_d], scalar1=midt[:, 0:1], scalar2=0.0, op0=ALU.is_gt, op1=ALU.add,
                                                           accum_out=stt[:, 4:5]),
                          reads=[score_b, mid_b], writes=[junk_b, stt_b])
                    if m_a > 0:
                        kb.op("dve", lambda e: e.scalar_tensor_tensor(out=stt[:, 4:5], in0=sta[:, 0:1], scalar=-0.5, in1=stt[:, 4:5], op0=ALU.mult, op1=ALU.add),
                              reads=[sta_b, stt_b], writes=[stt_b])
                    kb.op("dve", lambda e: e.tensor_scalar(out=stt[:, 5:6], in0=stt[:, 4:5], scalar1=255.5 - 0.5 * m_a, scalar2=wt[:, it:it + 1], op0=ALU.is_gt, op1=ALU.mult),
                          reads=[stt_b, wt_b], writes=[stt_b])
                    kb.op("dve", lambda e: e.scalar_tensor_tensor(out=midt[:, 0:1], in0=midt[:, 0:1], scalar=wt[:, it + 1:it + 2], in1=stt[:, 5:6],
                                                                  op0=ALU.subtract, op1=ALU.add),
                          reads=[stt_b, wt_b, mid_b], writes=[mid_b])
                return u
            if i >= 2:
                for it in range(NIT):
                    units.append(bis_unit(it))

            def fin_unit():
                if i >= 2:
                    kb.op("dve", lambda e: e.tensor_tensor(out=midt[:, 0:1], in0=midt[:, 0:1], in1=wt[:, NIT:NIT + 1], op=ALU.subtract), reads=[mid_b, wt_b], writes=[mid_b])
                kb.op("dve", lambda e: e.tensor_scalar(out=maskb[:, 0:n], in0=score[:, 0:n], scalar1=midt[:, 0:1], scalar2=None, op0=ALU.is_gt),
                      reads=[score_b, mid_b], writes=[maskb_b])
            units.append(fin_unit)

            def tr_unit(j0, nj):
                def u():
                    for jj in range(nj):
                        j = j0 + jj
                        kb.op("pe", lambda e: e.transpose(out=psT[:, jj, :], in_=maskb[:, j * 128:(j + 1) * 128], identity=ident[:]),
                              reads=[maskb_b, ident_b], writes=[psT_b])
                    kb.op("act", lambda e: e.activation(out=maskT2[mb][:, j0:j0 + nj, :], in_=psT[:, 0:nj, :], func=AF.Identity, scale=MASKB, bias=-MASKB),
                          reads=[psT_b], writes=[maskT2_b[mb]])
                return u
            for j0 in range(0, i + 1, 8):
                units.append(tr_unit(j0, min(8, i + 1 - j0)))
            return units

        tiles = list(range(NT) if qtiles is None else qtiles)
        for u in mask_units(tiles[0], 0):
            u()
        for ti, i in enumerate(tiles):
            n = (i + 1) * 128
            qsl = slice(i * 128, n)
            mb = ti % 2
            nxt = mask_units(tiles[ti + 1], (ti + 1) % 2) if ti + 1 < len(tiles) else []
            n_idx = 4 * len(range(0, (tiles[ti + 1] + 1) * 128, 512)) if ti + 1 < len(tiles) else 0
            R.pipe.set_bg(nxt, 2 * (i + 1), burst=4, nburst=n_idx)
            for hk in range(2):
                attn_tile(kb, R, hk, qsl, list(range(i + 1)), K, Q, V, lambda j, mb=mb: [(ident[:], maskT2[mb][:, j, :], [ident_b, maskT2_b[mb]])],
                          post=lambda o, o_b, hk=hk, qsl=qsl: kb.dma("sp", OTd[0:64, hk * 4:(hk + 1) * 4, qsl],
                                                                      o[:].rearrange("p (g q) -> p g q", g=4), reads=[o_b]))
            R.pipe.flush()
            R.pipe.flush_bg()
        kb.barrier()


def phase_swa(nc, kb, l, cin, sinks, QTd, KTd, Vtok, OTd, qtiles=None):
    with ExitStack() as es:
        S = _Scope(nc, es)
        Q, K, V = _load_qkv(nc, kb, S, "b", QTd, KTd, Vtok, 1)
        R = AttnRes(nc, kb, S)
        md = S.sb("md", [128, 128], BF16); md_b = kb.buf(dma=True)
        kb.dma("sp", md[:], cin["c_mdiagb"], writes=[md_b])
        mp = S.sb("mp", [128, 128], BF16); mp_b = kb.buf(dma=True)
        kb.dma("sp", mp[:], cin["c_mprevb"], writes=[mp_b])
        ident = S.sb("ident", [128, 128], BF16); ident_b = kb.buf(dma=True)
        kb.dma("sp", ident[:], cin["c_ident"], writes=[ident_b])
        sk = S.sb("sk", [65, 8], F32); sk_b = kb.buf(dma=True)
        kb.dma("sp", sk[64:65, :], sinks[l:l + 1, :], writes=[sk_b])
        kb.op("act", lambda e: e.activation(out=sk[64:65, :], in_=sk[64:65, :], func=AF.Exp), reads=[sk_b], writes=[sk_b])
        esb = S.sb("esb", [65, 8, 128], F32); esb_b = kb.buf()
        kb.op("dve", lambda e: e.tensor_copy(out=esb[64:65, :, :], in_=sk[64:65, :].unsqueeze(2).to_broadcast([1, 8, 128])), reads=[sk_b], writes=[esb_b])
        for i in (range(NT) if qtiles is None else qtiles):
            qsl = slice(i * 128, (i + 1) * 128)
            klist = [j for j in (i - 1, i) if j >= 0]
            for hk in range(2):
                attn_tile(kb, R, hk, qsl, klist, K, Q, V,
                          lambda j, i=i: [(ident[:], md[:], [ident_b, md_b])] if j == i else [(ident[:], mp[:], [ident_b, mp_b])],
                          extra_den=(esb[64:65, hk * 4:(hk + 1) * 4, :].rearrange("p g q -> p (g q)"), esb_b),
                          post=lambda o, o_b, hk=hk, qsl=qsl: kb.dma("sp", OTd[0:64, hk * 4:(hk + 1) * 4, qsl],
                                                                      o[:].rearrange("p (g q) -> p g q", g=4), reads=[o_b]))
        R.pipe.flush()
        kb.barrier()


def phase_cmp(nc, kb, l, KcTd, VcTd, peT, w_ck1, w_ck2, w_cv1, w_cv2, KcmpT, Vcmp):
    with ExitStack() as es:
        S = _Scope(nc, es)
        xc = [S.sb("xc%d" % i, [128, T], BF16) for i in range(2)]
        xc_b = kb.bufs(2, dma=True)
        kb.dma("sp", xc[0][:], KcTd, writes=[xc_b[0]])
        kb.dma("sp", xc[1][:], VcTd, writes=[xc_b[1]])
        w1 = [S.sb("w1_%d" % i, [128, 32, 256], BF16) for i in range(2)]
        w1_b = kb.bufs(2, dma="sw")
        pe = [S.sb("pe%d" % i, [128, 32], BF16) for i in range(2)]
        pe_b = kb.bufs(2, dma="sw")
        for si, wsrc in enumerate([w_ck1, w_cv1]):
            for half in range(2):
                kb.dma("pool", w1[si][half * 64:(half + 1) * 64, :, :], wsrc[l].rearrange("(l d) j -> d l j", d=64), writes=[w1_b[si]])
            kb.dma("pool", pe[si][:], peT[l, si], writes=[pe_b[si]])
        w2k = S.sb("w2k", [128, 2, 128], BF16); w2k_b = kb.buf(dma="sw")
        for half in range(2):
            kb.dma("pool", w2k[:, :, half * 64:(half + 1) * 64], w_ck2[l].rearrange("(jc j) d -> j jc d", j=128), writes=[w2k_b])
        w2v = S.sb("w2v", [128, 2, 64], BF16); w2v_b = kb.buf(dma="sw")
        kb.dma("pool", w2v[:], w_cv2[l].rearrange("(jc j) d -> j jc d", j=128), writes=[w2v_b])
        psB = S.ps("psB"); psB_b = kb.buf(excl=True)
        psH = [S.ps("psH%d" % i) for i in range(2)]; psH_b = kb.bufs(2, excl=True)
        psK = S.ps("psK"); psK_b = kb.buf(excl=True)
        b1s = S.sb("b1s", [128, 2], F32); b1s_b = kb.buf()
        hidb = S.sb("hidb", [128, 2, 256], BF16); hidb_b = kb.buf()
        kb.op("dve", lambda e: e.memset(hidb[:], 0.0), writes=[hidb_b])
        kcmp = S.sb("kcmp", [128, 256], BF16); kcmp_b = kb.buf(dma=True)
        kb.op("dve", lambda e: e.memset(kcmp[:], 0.0), writes=[kcmp_b])
        vcmp = S.sb("vcmp", [128, 2, 2, 64], BF16); vcmp_b = kb.buf(dma=True)
        kb.op("dve", lambda e: e.memset(vcmp[:], 0.0), writes=[vcmp_b])
        for si in range(2):
            for jc in range(2):
                for ll in range(32):
                    kb.op("pe", lambda e: e.matmul(psB[:, jc:jc + 1], lhsT=w1[si][0:64, ll, jc * 128:(jc + 1) * 128], rhs=pe[si][0:64, ll:ll + 1],
                                                   start=(ll == 0), stop=(ll == 31)),
                          reads=[w1_b[si], pe_b[si]], writes=[psB_b])
            kb.op("dve", lambda e: e.tensor_copy(out=b1s[:], in_=psB[:, 0:2]), reads=[psB_b], writes=[b1s_b])
            for hk in range(2):
                hs = slice(hk * 64, (hk + 1) * 64)
                for jc in range(2):
                    for ll in range(32):
                        kb.op("pe", lambda e: e.matmul(psH[jc][:, 0:255], lhsT=w1[si][hs, ll, jc * 128:(jc + 1) * 128],
                                                       rhs=xc[si][hs, ll:ll + 16 * 254 + 1:16], start=(ll == 0), stop=(ll == 31)),
                              reads=[w1_b[si], xc_b[si]], writes=[psH_b[jc]])
                    kb.op("act", lambda e: e.activation(out=hidb[:, jc, 0:255], in_=psH[jc][:, 0:255], func=AF.Relu, bias=b1s[:, jc:jc + 1], scale=1.0),
                          reads=[psH_b[jc], b1s_b], writes=[hidb_b])
                if si == 0:
                    for jc in range(2):
                        kb.op("pe", lambda e: e.matmul(psK[:, 0:255], lhsT=w2k[:, jc, :], rhs=hidb[:, jc, 0:255], start=(jc == 0), stop=(jc == 1)),
                              reads=[w2k_b, hidb_b], writes=[psK_b])
                    kb.op("act", lambda e: e.activation(out=kcmp[hs, 0:255], in_=psK[hs, 0:255], func=AF.Copy), reads=[psK_b], writes=[kcmp_b])
                else:
                    for ct in range(2):
                        nct = 128 if ct == 0 else 127
                        for jc in range(2):
                            kb.op("pe", lambda e: e.matmul(psK[0:nct, 0:64], lhsT=hidb[:, jc, ct * 128:ct * 128 + nct], rhs=w2v[:, jc, :],
                                                           start=(jc == 0), stop=(jc == 1)),
                                  reads=[w2v_b, hidb_b], writes=[psK_b])
                        kb.op("act", lambda e: e.activation(out=vcmp[0:nct, ct, hk, :], in_=psK[0:nct, 0:64], func=AF.Copy), reads=[psK_b], writes=[vcmp_b])
        kb.dma("sp", KcmpT, kcmp[:], reads=[kcmp_b])
        kb.dma("sp", Vcmp, vcmp[:], reads=[vcmp_b])
        kb.barrier()


def phase_nsa(nc, kb, l, cin, QTd, KsTd, KwTd, Vtok, KcmpT, Vcmp, sgcT, OTd, qtiles=None):
    with ExitStack() as es:
        S = _Scope(nc, es)
        Q, Ks, Vs = _load_qkv(nc, kb, S, "cs", QTd, KsTd, Vtok, 2)
        kw = S.sb("k_w", [128, T], BF16); kw_b = kb.buf(dma=True)
        kb.dma("sp", kw[:], KwTd, writes=[kw_b])
        Kw = (kw, kw_b)
        vw = S.sb("v_w", [128, NT, 2, 65], BF16); vw_b = kb.buf(dma=True)
        kb.op("pool", lambda e: e.memset(vw[:], 1.0), writes=[vw_b])
        for hk in range(2):
            kb.dma("sp", vw[:, :, hk, 0:64], Vtok[:, 3 * 128 + hk * 64: 3 * 128 + hk * 64 + 64].rearrange("(kt p) d -> p kt d", p=128), writes=[vw_b])
        Vw = (vw, vw_b)
        kcmp = S.sb("kcmp", [128, 256], BF16); kcmp_b = kb.buf(dma=True)
        kb.dma("sp", kcmp[:], KcmpT, writes=[kcmp_b])
        vcx = S.sb("vcx", [128, 2, 2, 65], BF16); vcx_b = kb.buf(dma=True)
        kb.op("pool", lambda e: e.memset(vcx[:], 1.0), writes=[vcx_b])
        kb.dma("sp", vcx[:, :, :, 0:64], Vcmp, writes=[vcx_b])
        ovx = S.sb("ovx", [128, 2, 65], BF16); ovx_b = kb.buf(dma=True)
        kb.op("pool", lambda e: e.memset(ovx[:], 1.0), writes=[ovx_b])
        kb.dma("sp", ovx[:, :, 0:64], cin["c_ovl"], writes=[ovx_b])
        mcmp = S.sb("mcmp", [128, 2, T], BF16); mcmp_b = kb.buf(dma=True)
        for ct in range(2):
            kb.dma("sp", mcmp[:, ct, :], cin["c_mcmp"][:, ct, :], writes=[mcmp_b])
        kbs = [S.sb("kbs%d" % i, [128, T], BF16) for i in range(2)]; kbs_b = kb.bufs(2, dma=True)
        for hk_ in range(2):
            hs_ = slice(hk_ * 64, (hk_ + 1) * 64)
            oh_ = slice((1 - hk_) * 64, (2 - hk_) * 64)
            kb.dma("sp", kbs[hk_][hs_, :], KsTd[hs_, :], writes=[kbs_b[hk_]])
            kb.dma("sp", kbs[hk_][oh_, :], cin["c_bsel"], writes=[kbs_b[hk_]])
        QB = [S.sb("QB%d" % i, [128, 4, 128], BF16) for i in range(2)]; QB_b = kb.bufs(2)
        f1 = S.sb("f1", [128, NT, 64], F32); f1_b = kb.buf(dma=True)
        kb.dma("sp", f1[:], cin["c_f1"], writes=[f1_b])
        f2 = S.sb("f2", [128, NT, 64], F32); f2_b = kb.buf(dma=True)
        kb.dma("sp", f2[:], cin["c_f2"], writes=[f2_b])
        sgc = S.sb("sgc", [32, T], F32); sgc_b = kb.buf(dma=True)
        kb.dma("sp", sgc[:], sgcT, writes=[sgc_b])
        gsel = S.sb("gsel", [32, 24, 64], F32); gsel_b = kb.buf(dma=True)
        kb.dma("sp", gsel[:], cin["c_gsel"], writes=[gsel_b])
        md = S.sb("md", [128, 128], BF16); md_b = kb.buf(dma=True)
        kb.dma("sp", md[:], cin["c_mdiagb"], writes=[md_b])
        mp = S.sb("mp", [128, 128], BF16); mp_b = kb.buf(dma=True)
        kb.dma("sp", mp[:], cin["c_mprevb"], writes=[mp_b])
        ident = S.sb("ident", [128, 128], BF16); ident_b = kb.buf(dma=True)
        kb.dma("sp", ident[:], cin["c_ident"], writes=[ident_b])
        R = AttnRes(nc, kb, S, out_dt=F32)
        psP = S.ps("psP"); psP_b = kb.buf(excl=True)
        psX = S.ps("psX"); psX_b = kb.buf(excl=True)
        psXT = S.ps("psXT", [128, 8, 128], BF16); psXT_b = kb.buf(excl=True)
        Emc = [S.sb("Emc%d" % i, [128, 512], BF16) for i in range(2)]; Emc_b = kb.bufs(2)
        Ec = [S.sb("Ec%d" % i, [128, 512], BF16) for i in range(2)]; Ec_b = kb.bufs(2)
        pp = S.sb("pp", [128, 4, 65], F32); pp_b = kb.buf()
        rec = S.sb("rec", [128, 4], F32); rec_b = kb.buf()
        imp = [S.sb("imp%d" % i, [128, 64], F32) for i in range(2)]; imp_b = kb.bufs(2)
        m8 = S.sb("m8", [128, 16], F32); m8_b = kb.buf()
        imp3 = S.sb("imp3", [128, 64], F32); imp3_b = kb.buf()
        bm2 = [S.sb("bm%d" % i, [128, 128], BF16) for i in range(2)]; bm2_b = kb.bufs(2)
        for i_ in range(2):
            kb.op("pool", lambda e: e.memset(bm2[i_][:], 0.0), writes=[bm2_b[i_]])

        acc = [S.sb("acc%d" % i, [64, 512], F32) for i in range(2)]; acc_b = kb.bufs(2)
        tmpo = S.sb("tmpo", [64, 512], F32); tmpo_b = kb.buf()
        tmpc = S.sb("tmpc", [64, 512], F32); tmpc_b = kb.buf()
        oTc = S.sb("oTc", [65, 512], F32); oTc_b = kb.buf()
        rrc = S.sb("rrc", [65, 512], F32); rrc_b = kb.buf()
        ocb = [S.sb("ocb%d" % i, [64, 512], BF16) for i in range(2)]; ocb_b = kb.bufs(2, dma=True)

        def gate_acc(hk, br, o, o_b, first, qsl):
            for g in range(4):
                r = (hk * 4 + g) * 3 + br
                kb.op("pe", lambda e: e.matmul(psX[0:64, g * 128:(g + 1) * 128], lhsT=gsel[:, r, :], rhs=sgc[:, qsl], start=True, stop=True),
                      reads=[gsel_b, sgc_b], writes=[psX_b])
            if first:
                kb.op("dve", lambda e: e.tensor_tensor(out=acc[hk][:], in0=o[:], in1=psX[0:64, :], op=ALU.mult), reads=[o_b, psX_b], writes=[acc_b[hk]])
            else:
                kb.op("dve", lambda e: e.tensor_tensor(out=tmpo[:], in0=o[:], in1=psX[0:64, :], op=ALU.mult), reads=[o_b, psX_b], writes=[tmpo_b])
                kb.op("pool", lambda e: e.tensor_tensor(out=acc[hk][:], in0=acc[hk][:], in1=tmpo[:], op=ALU.add), reads=[tmpo_b, acc_b[hk]], writes=[acc_b[hk]])

        def sel_units(i, hk):
            qsl = slice(i * 128, (i + 1) * 128)
            nct = 1 if 8 * i + 6 < 128 else 2
            hs = slice(hk * 64, (hk + 1) * 64)
            oh = slice((1 - hk) * 64, (2 - hk) * 64)
            units = []

            def u_scores(ct):
                def u():
                    pi = R.pcnt % 2
                    R.pcnt += 1
                    kb.op("pe", lambda e: e.matmul(R.psS[pi][:].rearrange("p (g q) -> p g q", g=4), lhsT=kcmp[hs, ct * 128:(ct + 1) * 128],
                                                   rhs=Q[0][hs, :, qsl], start=True, stop=True),
                          reads=[kcmp_b, Q[1]], writes=[R.psS_b[pi]])
                    kb.op("act", lambda e: e.activation(out=Ec[ct][:], in_=R.psS[pi][:], func=AF.Exp, scale=0.125), reads=[R.psS_b[pi]], writes=[Ec_b[ct]])
                    kb.op("pool", lambda e: e.tensor_tensor(out=Emc[ct][:].rearrange("p (g q) -> p g q", g=4),
                                                            in0=Ec[ct][:].rearrange("p (g q) -> p g q", g=4),
                                                            in1=mcmp[:, ct, qsl].unsqueeze(1).to_broadcast([128, 4, 128]), op=ALU.mult),
                          reads=[Ec_b[ct], mcmp_b], writes=[Emc_b[ct]])
                return u
            for ct in range(nct):
                units.append(u_scores(ct))

            def u_pv():
                for ct in range(nct):
                    kb.op("pe", lambda e: e.matmul(psP[0:65, :], lhsT=vcx[:, ct, hk, :], rhs=Emc[ct][:], start=(ct == 0), stop=(ct == nct - 1)),
                          reads=[vcx_b, Emc_b[ct]], writes=[psP_b])
                kb.op("act", lambda e: e.activation(out=oTc[0:65, :], in_=psP[0:65, :], func=AF.Copy), reads=[psP_b], writes=[oTc_b])
                for g in range(4):
                    for ct in range(nct):
                        kb.op("pe", lambda e: e.matmul(psP[:, g * 65:(g + 1) * 65], lhsT=Emc[ct][:, g * 128:(g + 1) * 128], rhs=ovx[:, ct, :],
                                                       start=(ct == 0), stop=(ct == nct - 1)),
                              reads=[Emc_b[ct], ovx_b], writes=[psP_b])
                kb.op("act", lambda e: e.activation(out=pp[:].rearrange("p g c -> p (g c)"), in_=psP[:, 0:260], func=AF.Copy), reads=[psP_b], writes=[pp_b])
            units.append(u_pv)

            def u_ocmp():
                kb.op("dve", lambda e: e.tensor_scalar(out=oTc[64:65, :], in0=oTc[64:65, :], scalar1=1e-30, scalar2=None, op0=ALU.max), reads=[oTc_b], writes=[oTc_b])
                kb.op("act", lambda e: e.activation(out=rrc[64:65, :], in_=oTc[64:65, :], func=AF.Ln), reads=[oTc_b], writes=[rrc_b])
                kb.op("act", lambda e: e.activation(out=rrc[64:65, :], in_=rrc[64:65, :], func=AF.Exp, scale=-1.0), reads=[rrc_b], writes=[rrc_b])
                kb.op("pe", lambda e: e.matmul(psX[0:64, :], lhsT=R.ones32[64:65, 0:64], rhs=rrc[64:65, :], start=True, stop=True),
                      reads=[R.ones32_b, rrc_b], writes=[psX_b])
                kb.op("dve", lambda e: e.tensor_tensor(out=tmpc[:], in0=oTc[0:64, :], in1=psX[0:64, :], op=ALU.mult), reads=[oTc_b, psX_b], writes=[tmpc_b])
                gate_acc(hk, 0, tmpc, tmpc_b, True, qsl)
            units.append(u_ocmp)

            def u_imp():
                kb.op("dve", lambda e: e.tensor_scalar(out=rec[:], in0=pp[:, :, 64], scalar1=1e-30, scalar2=None, op0=ALU.max), reads=[pp_b], writes=[rec_b])
                kb.op("dve", lambda e: e.reciprocal(out=rec[:], in_=rec[:]), reads=[rec_b], writes=[rec_b])
                kb.op("dve", lambda e: e.tensor_scalar(out=imp[hk][:], in0=pp[:, 0, 0:64], scalar1=rec[:, 0:1], scalar2=None, op0=ALU.mult), reads=[pp_b, rec_b], writes=[imp_b[hk]])
                for g in range(1, 4):
                    kb.op("dve", lambda e: e.scalar_tensor_tensor(out=imp[hk][:], in0=pp[:, g, 0:64], scalar=rec[:, g:g + 1], in1=imp[hk][:], op0=ALU.mult, op1=ALU.add),
                          reads=[pp_b, rec_b, imp_b[hk]], writes=[imp_b[hk]])
                kb.op("dve", lambda e: e.tensor_tensor(out=imp[hk][:], in0=imp[hk][:], in1=f1[:, i, :], op=ALU.max), reads=[imp_b[hk], f1_b], writes=[imp_b[hk]])
                kb.op("dve", lambda e: e.tensor_tensor(out=imp[hk][:], in0=imp[hk][:], in1=f2[:, i, :], op=ALU.min), reads=[imp_b[hk], f2_b], writes=[imp_b[hk]])
            units.append(u_imp)

            def u_top():
                kb.op("dve", lambda e: e.max(out=m8[:, 0:8], in_=imp[hk][:]), reads=[imp_b[hk]], writes=[m8_b])
                kb.op("dve", lambda e: e.match_replace(out=imp3[:], in_to_replace=m8[:, 0:8], in_values=imp[hk][:], imm_value=-3e38), reads=[imp_b[hk], m8_b], writes=[imp3_b])
                kb.op("dve", lambda e: e.max(out=m8[:, 8:16], in_=imp3[:]), reads=[imp3_b], writes=[m8_b])
                kb.op("dve", lambda e: e.tensor_scalar(out=bm2[hk][:, oh], in0=imp[hk][:], scalar1=m8[:, 15:16], scalar2=None, op0=ALU.is_ge),
                      reads=[imp_b[hk], m8_b], writes=[bm2_b[hk]])
            units.append(u_top)

            def u_tr():
                kb.op("pe", lambda e: e.transpose(out=psXT[:, 0, :], in_=bm2[hk][:], identity=ident[:]), reads=[bm2_b[hk], ident_b], writes=[psXT_b])
                kb.op("act", lambda e: e.activation(out=QB[hk][oh, :, :], in_=psXT[oh, 0, :].unsqueeze(1).to_broadcast([64, 4, 128]),
                                                    func=AF.Identity, scale=MASKB, bias=-MASKB),
                      reads=[psXT_b], writes=[QB_b[hk]])
                kb.op("pool", lambda e: e.tensor_copy(out=QB[hk][hs, :, :], in_=Q[0][hs, :, qsl]), reads=[Q[1]], writes=[QB_b[hk]])
            units.append(u_tr)
            return units

        order = [(i, hk) for i in (range(NT) if qtiles is None else qtiles) for hk in range(2)]
        for u in sel_units(*order[0]):
            u()
        for oi, (i, hk) in enumerate(order):
            qsl = slice(i * 128, (i + 1) * 128)
            wl = list(range(max(0, i - 4), i + 1))
            nxt = sel_units(*order[oi + 1]) if oi + 1 < len(order) else []
            R.pipe.set_bg(nxt, (i + 1) + len(wl))

            def slc_mask(j, i=i, hk=hk):
                return [(ident[:], md[:], [ident_b, md_b])] if j == i else None

            def slc_st(j, hk=hk):
                return kbs[hk][:, j * 128:(j + 1) * 128], QB[hk][:], [kbs_b[hk], QB_b[hk]]

            attn_tile(kb, R, hk, qsl, list(range(i + 1)), Ks, Q, Vs, slc_mask, st_override=slc_st,
                      post=lambda o, o_b, hk=hk, qsl=qsl: gate_acc(hk, 1, o, o_b, False, qsl))

            def win_post(o, o_b, hk=hk, qsl=qsl):
                gate_acc(hk, 2, o, o_b, False, qsl)
                kb.op("act", lambda e: e.activation(out=ocb[hk][:], in_=acc[hk][:], func=AF.Copy), reads=[acc_b[hk]], writes=[ocb_b[hk]])
                kb.dma("sp", OTd[0:64, hk * 4:(hk + 1) * 4, qsl], ocb[hk][:].rearrange("p (g q) -> p g q", g=4), reads=[ocb_b[hk]])

            attn_tile(kb, R, hk, qsl, wl, Kw, Q, Vw,
                      lambda j, i=i: [(ident[:], md[:], [ident_b, md_b])] if j == i else ([(ident[:], mp[:], [ident_b, mp_b])] if j == i - 4 else None),
                      post=win_post)
            R.pipe.flush()
            R.pipe.flush_bg()
        kb.barrier()


def phase_merge(nc, kb, l, x_src, x_dst, OTd, sgT, w_branch, w_out):
    with ExitStack() as es:
        S = _Scope(nc, es)
        wb = S.sb("wb", [128, 3, 4, D], BF16); wb_b = kb.buf(dma="sw")
        for n in range(3):
            kb.dma("pool", wb[:, n, :, :], w_branch[l, n].rearrange("(c p) o -> p c o", p=128), writes=[wb_b])
        wo = S.sb("wo", [128, 8, D], BF16); wo_b = kb.buf(dma="sw")
        for kc in range(8):
            kb.dma("pool", wo[:, kc, :], w_out[l, kc * 128:(kc + 1) * 128, :], writes=[wo_b])
        sg = [S.sb("sg%d" % i, [128, 24, 512], BF16) for i in range(2)]; sg_b = kb.bufs(2, dma=True)
        ot = [S.sb("ot%d" % i, [128, 3, 4, 512], BF16) for i in range(2)]; ot_b = kb.bufs(2, dma=True)
        xt = [S.sb("xt%d" % i, [128, D], F32) for i in range(2)]; xt_b = kb.bufs(2, dma=True)
        mT = S.sb("mT", [128, 8, 512], BF16); mT_b = kb.buf()
        t0a = [S.sb("t0_%d" % i, [128, 512], F32) for i in range(3)]; t0a_b = kb.bufs(3)
        psY6 = [S.ps("psY%d" % i) for i in range(6)]; psY6_b = kb.bufs(6, excl=True)
        psXo = [S.ps("psXo%d" % i) for i in range(2)]; psXo_b = kb.bufs(2, excl=True)
        t06 = [S.sb("t0b_%d" % i, [128, 512], F32) for i in range(3)]; t06_b = kb.bufs(3)
        xcnt = 0
        def load_group(tg_):
            gi_ = tg_ % 2
            tsl_ = slice(tg_ * 512, (tg_ + 1) * 512)
            for c in range(24):
                kb.dma("sp", sg[gi_][:, c, :], sgT[:, c, tsl_], writes=[sg_b[gi_]])
            for n in range(3):
                for par in range(2):
                    kb.dma("sp", ot[gi_][par * 64:(par + 1) * 64, n, :, :], OTd[n][0:64, par::2, tsl_], writes=[ot_b[gi_]])

        load_group(0)
        for tg in range(8):
            gi = tg % 2
            tsl = slice(tg * 512, (tg + 1) * 512)
            if tg + 1 < 8:
                load_group(tg + 1)
            for oc in range(8):
                psY, psY_b = psY6[(oc % 2) * 3:(oc % 2) * 3 + 3], psY6_b[(oc % 2) * 3:(oc % 2) * 3 + 3]
                t0, t0_b = (t0a, t0a_b) if oc % 2 == 0 else (t06, t06_b)
                for n in range(3):
                    for c in range(4):
                        kb.op("pe", lambda e: e.matmul(psY[n][:], lhsT=wb[:, n, c, oc * 128:(oc + 1) * 128], rhs=ot[gi][:, n, c, :], start=(c == 0), stop=(c == 3)),
                              reads=[wb_b, ot_b[gi]], writes=[psY_b[n]])
                    kb.op("dve", lambda e: e.tensor_tensor(out=t0[n][:], in0=psY[n][:], in1=sg[gi][:, n * 8 + oc, :], op=ALU.mult),
                          reads=[psY_b[n], sg_b[gi]], writes=[t0_b[n]])
                kb.op("pool", lambda e: e.tensor_tensor(out=t0[0][:], in0=t0[0][:], in1=t0[1][:], op=ALU.add), reads=[t0_b[0], t0_b[1]], writes=[t0_b[0]])
                kb.op("pool", lambda e: e.tensor_tensor(out=mT[:, oc, :], in0=t0[0][:], in1=t0[2][:], op=ALU.add), reads=[t0_b[0], t0_b[2]], writes=[mT_b])
            for tt in range(4):
                xi = xcnt % 2
                xcnt += 1
                rows = slice(tg * 512 + tt * 128, tg * 512 + (tt + 1) * 128)
                kb.dma("sp", xt[xi][:], x_src[rows, :], writes=[xt_b[xi]])
                for half in range(2):
                    pb = half
                    for kc in range(8):
                        kb.op("pe", lambda e: e.matmul(psXo[pb][:], lhsT=mT[:, kc, tt * 128:(tt + 1) * 128], rhs=wo[:, kc, half * 512:(half + 1) * 512],
                                                       start=(kc == 0), stop=(kc == 7)),
                              reads=[mT_b, wo_b], writes=[psXo_b[pb]])
                    kb.op("dve", lambda e: e.tensor_tensor(out=xt[xi][:, half * 512:(half + 1) * 512], in0=xt[xi][:, half * 512:(half + 1) * 512],
                                                           in1=psXo[pb][:], op=ALU.add),
                          reads=[xt_b[xi], psXo_b[pb]], writes=[xt_b[xi]])
                kb.dma("sp", x_dst[rows, :], xt[xi][:], reads=[xt_b[xi]])
        kb.barrier()


def phase_mlp(nc, kb, l, x_src, x_dst, norm_mlp, w_up, w_down, cin):
    with ExitStack() as es:
        S = _Scope(nc, es)
        wu = S.sb("wu", [128, 8, D_FF], BF16); wu_b = kb.buf(dma="sw")
        for kc in range(8):
            kb.dma("pool", wu[:, kc, :], w_up[l, kc * 128:(kc + 1) * 128, :], writes=[wu_b])
        wd = S.sb("wd", [128, 32, D], BF16); wd_b = kb.buf(dma="sw")
        for c4 in range(8):
            kb.dma("pool", wd[:, c4 * 4:(c4 + 1) * 4, :], w_down[l, c4 * 512:(c4 + 1) * 512, :].rearrange("(c p) o -> p c o", p=128), writes=[wd_b])
        gt = S.sb("gt", [128, D], F32); gt_b = kb.buf(dma=True)
        kb.dma("sp", gt[:], norm_mlp[l].partition_broadcast(128), writes=[gt_b])
        ident = S.sb("ident", [128, 128], BF16); ident_b = kb.buf(dma=True)
        kb.dma("sp", ident[:], cin["c_ident"], writes=[ident_b])
        xt = [S.sb("xt%d" % i, [128, D], F32) for i in range(4)]; xt_b = kb.bufs(4, dma=True)
        junk = S.sb("junk", [128, D], BF16); junk_b = kb.buf()
        st = [S.sb("st%d" % i, [128, 4], F32) for i in range(2)]; st_b = kb.bufs(2)
        hb = [S.sb("hb%d" % i, [128, D], BF16) for i in range(2)]; hb_b = kb.bufs(2)
        h2T = S.sb("h2T", [128, 8, 256], BF16); h2T_b = kb.buf()
        uT = S.sb("uT", [128, 32, 256], BF16); uT_b = kb.buf()
        rr = [S.sb("rr%d" % i, [128, 256], F32) for i in range(2)]; rr_b = kb.bufs(2)
        pst = [S.ps("pst%d" % i, [128, 8, 128], BF16) for i in range(2)]; pst_b = kb.bufs(2, excl=True)
        psU = [S.ps("psU%d" % i) for i in range(2)]; psU_b = kb.bufs(2, excl=True)
        psDn = [S.ps("psDn%d" % i) for i in range(4)]; psDn_b = kb.bufs(4, excl=True)
        ucnt = 0
        for t2 in range(T // 256):
            for tt in range(2):
                xi = (t2 % 2) * 2 + tt
                i = tt
                rows = slice(t2 * 256 + tt * 128, t2 * 256 + (tt + 1) * 128)
                kb.dma("sp", xt[xi][:], x_src[rows, :], writes=[xt_b[xi]])
                kb.op("act", lambda e: e.activation(out=junk[:], in_=xt[xi][:], func=AF.Square, accum_out=st[i][:, 0:1]),
                      reads=[xt_b[xi]], writes=[junk_b, st_b[i]])
                kb.op("act", lambda e: e.activation(out=st[i][:, 1:2], in_=st[i][:, 0:1], func=AF.Sqrt, bias=EPS, scale=1.0 / D), reads=[st_b[i]], writes=[st_b[i]])
                kb.op("dve", lambda e: e.reciprocal(out=st[i][:, 2:3], in_=st[i][:, 1:2]), reads=[st_b[i]], writes=[st_b[i]])
                kb.op("dve", lambda e: e.scalar_tensor_tensor(out=hb[i][:], in0=xt[xi][:], scalar=st[i][:, 2:3], in1=gt[:], op0=ALU.mult, op1=ALU.mult),
                      reads=[xt_b[xi], st_b[i], gt_b], writes=[hb_b[i]])
                for kc in range(8):
                    kb.op("pe", lambda e: e.transpose(out=pst[i][:, kc, :], in_=hb[i][:, kc * 128:(kc + 1) * 128], identity=ident[:]),
                          reads=[hb_b[i], ident_b], writes=[pst_b[i]])
                kb.op("act", lambda e: e.activation(out=h2T[:, :, tt * 128:(tt + 1) * 128], in_=pst[i][:], func=AF.Copy), reads=[pst_b[i]], writes=[h2T_b])
            for ffc in range(32):
                ui = ucnt % 2
                ucnt += 1
                for kc in range(8):
                    kb.op("pe", lambda e: e.matmul(psU[ui][:, 0:256], lhsT=wu[:, kc, ffc * 128:(ffc + 1) * 128], rhs=h2T[:, kc, :], start=(kc == 0), stop=(kc == 7)),
                          reads=[wu_b, h2T_b], writes=[psU_b[ui]])
                kb.op("act", lambda e: e.activation(out=rr[ui][:], in_=psU[ui][:, 0:256], func=AF.Relu), reads=[psU_b[ui]], writes=[rr_b[ui]])
                kb.op("pool", lambda e: e.tensor_tensor(out=uT[:, ffc, :], in0=rr[ui][:], in1=rr[ui][:], op=ALU.mult), reads=[rr_b[ui]], writes=[uT_b])
            for tt in range(2):
                xi = (t2 % 2) * 2 + tt
                rows = slice(t2 * 256 + tt * 128, t2 * 256 + (tt + 1) * 128)
                for half in range(2):
                    pb = tt * 2 + half
                    for ffc in range(32):
                        kb.op("pe", lambda e: e.matmul(psDn[pb][:], lhsT=uT[:, ffc, tt * 128:(tt + 1) * 128], rhs=wd[:, ffc, half * 512:(half + 1) * 512],
                                                       start=(ffc == 0), stop=(ffc == 31)),
                              reads=[uT_b, wd_b], writes=[psDn_b[pb]])
                    kb.op("dve", lambda e: e.tensor_tensor(out=xt[xi][:, half * 512:(half + 1) * 512], in0=xt[xi][:, half * 512:(half + 1) * 512],
                                                           in1=psDn[pb][:], op=ALU.add),
                          reads=[xt_b[xi], psDn_b[pb]], writes=[xt_b[xi]])
                kb.dma("sp", x_dst[rows, :], xt[xi][:], reads=[xt_b[xi]])
        kb.barrier()
```
